# Optimizing a Trainium2 kernel written in Bass

```python
import math
import jax, jax.numpy as jnp
from jax import lax
import numpy as np

D_MODEL = 2048
BATCH = 1
SEQ = 8192
DEPTH = 2

GRID_W = 64
CTX_LEN = 256
HEAD_DIM = 128
HG_HEADS = D_MODEL // (4 * HEAD_DIM)
HG_WIDTH = HG_HEADS * HEAD_DIM
HG_CHUNK = 64
GQA_HEADS = D_MODEL // (2 * HEAD_DIM)
GQA_KV_HEADS = GQA_HEADS // 4
GQA_WIDTH = GQA_HEADS * HEAD_DIM
GQA_KV_WIDTH = GQA_KV_HEADS * HEAD_DIM
DIFF_HEADS = D_MODEL // (4 * HEAD_DIM)
DIFF_QK_DIM = HEAD_DIM // 2
DIFF_WIDTH = DIFF_HEADS * HEAD_DIM
MIX_WIDTH = HG_WIDTH + GQA_WIDTH + DIFF_WIDTH
IN_SPLITS = (HG_WIDTH,) * 6 + (GQA_WIDTH, GQA_KV_WIDTH, GQA_KV_WIDTH) + (DIFF_WIDTH,) * 3
IN_WIDTH = sum(IN_SPLITS)
Q_BLOCK = 128
ROPE_THETA = 10000.0
N_GROUPS = 4
EXPERTS_PER_GROUP = 8
N_EXPERTS = N_GROUPS * EXPERTS_PER_GROUP
EXPERT_FF = 512
TOP_K = 2
N_MOD = 6
EPS = 1e-6
F32 = jnp.float32

kernel_name = "hybrid_hgrn2_gqa_diffattn_hmoe_dit"


def rms_norm(x, g):
    xf = x.astype(F32)
    y = xf * lax.rsqrt(jnp.mean(xf * xf, axis=-1, keepdims=True) + EPS)
    return (y * g.astype(F32)).astype(x.dtype)


def modulate(h, shift, scale):
    return h * (1 + scale) + shift


def split_cols(p):
    return jnp.split(p, np.cumsum(IN_SPLITS)[:-1].tolist(), axis=-1)


def to_heads(a, n):
    B, L, _ = a.shape
    return a.reshape(B, L, n, -1).transpose(0, 2, 1, 3)


def from_heads(a):
    B, H, L, d = a.shape
    return a.transpose(0, 2, 1, 3).reshape(B, L, H * d)


def axial_rope(n_tokens, dim):
    rows = n_tokens // GRID_W
    row = jnp.repeat(jnp.arange(rows, dtype=F32), GRID_W)
    col = jnp.tile(jnp.arange(GRID_W, dtype=F32), rows)
    axis_dim = dim // 2
    inv_freq = ROPE_THETA ** (-jnp.arange(0, axis_dim, 2, dtype=F32) / axis_dim)
    ang = jnp.concatenate([row[:, None] * inv_freq, col[:, None] * inv_freq], axis=-1)
    return jnp.cos(ang), jnp.sin(ang)


def apply_rope(x, cos, sin):
    xf = x.astype(F32).reshape(*x.shape[:-1], -1, 2)
    x0, x1 = xf[..., 0], xf[..., 1]
    out = jnp.stack([x0 * cos - x1 * sin, x0 * sin + x1 * cos], axis=-1)
    return out.reshape(x.shape).astype(x.dtype)


def hgrn_log_forget(z, lb):
    lbf = lb.astype(F32)
    return jnp.logaddexp(jnp.log(lbf), jnp.log1p(-lbf) + jax.nn.log_sigmoid(z.astype(F32)))


def hgrn2_bidir_inputs(qf, qb, ff, fb, iv, lb):
    flip = lambda a: jnp.flip(a, axis=2)
    logf = jnp.concatenate([to_heads(hgrn_log_forget(ff, lb), HG_HEADS),
                            flip(to_heads(hgrn_log_forget(fb, lb), HG_HEADS))], axis=1)
    q = jnp.concatenate([to_heads(qf, HG_HEADS), flip(to_heads(qb, HG_HEADS))], axis=1)
    vh = to_heads(iv, HG_HEADS)
    v = jnp.concatenate([vh, flip(vh)], axis=1)
    k = -jnp.expm1(logf)
    return q, k, v, logf


def chunk_gated_scan(q, k, v, logf, s0):
    B, H, T, dk = q.shape
    dv = v.shape[-1]
    n = T // HG_CHUNK
    chunks = lambda a: a.reshape(B, H, n, HG_CHUNK, a.shape[-1]).transpose(2, 0, 1, 3, 4)
    order_mask = jnp.tril(jnp.ones((HG_CHUNK, HG_CHUNK), dtype=bool))[:, :, None]

    def step(state, inp):
        qc, kc, vc, gc = inp
        b = jnp.cumsum(gc.astype(F32), axis=-2)
        rel = jnp.where(order_mask, b[..., :, None, :] - b[..., None, :, :], -jnp.inf)
        intra = jnp.einsum('bhtd,bhsd,bhtsd->bhts', qc.astype(F32), kc.astype(F32), jnp.exp(rel))
        o = (jnp.einsum('bhts,bhse->bhte', intra, vc.astype(F32))
             + jnp.einsum('bhtd,bhde->bhte', qc * jnp.exp(b), state))
        b_end = b[..., -1:, :]
        new_state = (jnp.exp(b_end[..., 0, :])[..., None] * state
                     + jnp.einsum('bhsd,bhse->bhde', kc * jnp.exp(b_end - b), vc.astype(F32)))
        return new_state, o

    state, o = lax.scan(step, s0, (chunks(q), chunks(k), chunks(v), chunks(logf)))
    return o.transpose(1, 2, 0, 3, 4).reshape(B, H, T, dv), state


def hgrn2_readout(o, gate, norm_g):
    o = o[:, :HG_HEADS] + jnp.flip(o[:, HG_HEADS:], axis=2)
    return from_heads(rms_norm(o, norm_g)).astype(gate.dtype) * jax.nn.silu(gate)


def gqa_heads(q, k, v, qn, kn):
    return (rms_norm(to_heads(q, GQA_HEADS), qn), rms_norm(to_heads(k, GQA_KV_HEADS), kn),
            to_heads(v, GQA_KV_HEADS))


def gqa_attention(q, k, v):
    B, H, Tq, d = q.shape
    KV = k.shape[1]
    nb = Tq // Q_BLOCK
    qb = q.reshape(B, KV, H // KV, nb, Q_BLOCK, d).transpose(3, 0, 1, 2, 4, 5)

    def one_block(qblk):
        s = jnp.einsum('bkgqd,bksd->bkgqs', qblk, k).astype(F32) * (d ** -0.5)
        p = jax.nn.softmax(s, axis=-1)
        return jnp.einsum('bkgqs,bksd->bkgqd', p.astype(v.dtype), v)

    o = lax.map(one_block, qb)
    return from_heads(o.transpose(1, 2, 3, 0, 4, 5).reshape(B, H, Tq, d))


def diff_heads(q, k, v):
    B, L, _ = q.shape
    q = q.reshape(B, L, DIFF_HEADS, 2, DIFF_QK_DIM).transpose(3, 0, 2, 1, 4)
    k = k.reshape(B, L, DIFF_HEADS, 2, DIFF_QK_DIM).transpose(3, 0, 2, 1, 4)
    return q, k, to_heads(v, DIFF_HEADS)


def diff_attention(q, k, v, lam):
    _, B, H, Tq, d = q.shape
    nb = Tq // Q_BLOCK
    qb = q.reshape(2, B, H, nb, Q_BLOCK, d).transpose(3, 0, 1, 2, 4, 5)

    def one_block(qblk):
        s = jnp.einsum('jbhqd,jbhkd->jbhqk', qblk, k).astype(F32) * (d ** -0.5)
        p = jax.nn.softmax(s, axis=-1)
        w = p[0] - lam * p[1]
        return jnp.einsum('bhqk,bhkd->bhqd', w.astype(v.dtype), v)

    o = lax.map(one_block, qb)
    return o.transpose(1, 2, 0, 3, 4).reshape(B, H, Tq, v.shape[-1])


def diff_readout(o, norm_g, lam_init):
    return from_heads(rms_norm(o, norm_g) * (1 - lam_init))


def hier_moe(h, rgw, rgb, rew, reb, w_gate, w_up, w_down):
    N = h.shape[0]
    g_prob = jax.nn.softmax((h @ rgw + rgb).astype(F32), axis=-1)
    g_top, g_idx = lax.top_k(g_prob, 1)
    e_logits = (h @ rew + reb).astype(F32).reshape(N, N_GROUPS, EXPERTS_PER_GROUP)
    e_sel = jnp.take_along_axis(e_logits, g_idx[:, :, None], axis=1)[:, 0]
    e_top, e_idx = lax.top_k(jax.nn.softmax(e_sel, axis=-1), TOP_K)
    weights = g_top * e_top / jnp.sum(e_top, axis=-1, keepdims=True)
    expert_id = g_idx * EXPERTS_PER_GROUP + e_idx
    combine = jnp.sum(jax.nn.one_hot(expert_id, N_EXPERTS, dtype=F32) * weights[..., None], axis=1)
    y = jnp.zeros_like(h)
    for gi in range(N_GROUPS):
        sl = slice(gi * EXPERTS_PER_GROUP, (gi + 1) * EXPERTS_PER_GROUP)
        a = jax.nn.silu(jnp.einsum('nd,edf->enf', h, w_gate[sl])) * jnp.einsum('nd,edf->enf', h, w_up[sl])
        a = a * combine[:, sl].T[:, :, None].astype(a.dtype)
        y = y + jnp.einsum('enf,efd->nd', a, w_down[sl])
    return y


def layer(xl, xc, c, c_ctx, w_mod, b_mod, n1, n2, w_in, w_out, lb, hg_ng, qn, kn,
          lq1, lk1, lq2, lk2, dn, rgw, rgb, rew, reb, w_gate, w_up, w_down,
          layer_idx, ropes, update_ctx):
    B = xl.shape[0]
    D = xl.shape[-1]
    sh1, sc1, gt1, sh2, sc2, gt2 = jnp.split((jax.nn.silu(c) @ w_mod + b_mod)[:, None, :], N_MOD, axis=-1)
    csh1, csc1, cgt1, csh2, csc2, cgt2 = jnp.split(jax.nn.silu(c_ctx) @ w_mod + b_mod, N_MOD, axis=-1)
    (cos_a, sin_a), (cos_d, sin_d) = ropes

    pl = split_cols(modulate(rms_norm(xl, n1), sh1, sc1) @ w_in)
    pc = split_cols(modulate(rms_norm(xc, n1), csh1, csc1) @ w_in)

    s0 = jnp.zeros((B, 2 * HG_HEADS, HEAD_DIM, HEAD_DIM), F32)
    o_c, s_c = chunk_gated_scan(*hgrn2_bidir_inputs(*pc[:5], lb), s0)
    o_l, _ = chunk_gated_scan(*hgrn2_bidir_inputs(*pl[:5], lb), s_c)
    hg_l = hgrn2_readout(o_l, pl[5], hg_ng)

    qa_l, ka_l, va_l = gqa_heads(*pl[6:9], qn, kn)
    qa_l = apply_rope(qa_l, cos_a, sin_a)
    ka_l = apply_rope(ka_l, cos_a, sin_a)
    qa_c, ka_c, va_c = gqa_heads(*pc[6:9], qn, kn)
    att_l = gqa_attention(qa_l, jnp.concatenate([ka_c, ka_l], axis=2), jnp.concatenate([va_c, va_l], axis=2))

    qd_l, kd_l, vd_l = diff_heads(*pl[9:12])
    qd_l = apply_rope(qd_l, cos_d, sin_d)
    kd_l = apply_rope(kd_l, cos_d, sin_d)
    qd_c, kd_c, vd_c = diff_heads(*pc[9:12])
    lam_init = 0.8 - 0.6 * math.exp(-0.3 * layer_idx)
    lam = (jnp.exp(jnp.sum(lq1.astype(F32) * lk1.astype(F32)))
           - jnp.exp(jnp.sum(lq2.astype(F32) * lk2.astype(F32))) + lam_init)
    diff_l = diff_readout(diff_attention(qd_l, jnp.concatenate([kd_c, kd_l], axis=3),
                                         jnp.concatenate([vd_c, vd_l], axis=2), lam), dn, lam_init)

    xl = xl + gt1 * (jnp.concatenate([hg_l, att_l, diff_l], axis=-1) @ w_out)

    hl2 = modulate(rms_norm(xl, n2), sh2, sc2)
    if update_ctx:
        hg_c = hgrn2_readout(o_c, pc[5], hg_ng)
        att_c = gqa_attention(qa_c, ka_c, va_c)
        diff_c = diff_readout(diff_attention(qd_c, kd_c, vd_c, lam), dn, lam_init)
        xc = xc + cgt1 * (jnp.concatenate([hg_c, att_c, diff_c], axis=-1) @ w_out)
        hc2 = modulate(rms_norm(xc, n2), csh2, csc2)
        Lc = xc.shape[1]
        tokens = jnp.concatenate([hc2, hl2], axis=1)
        y = hier_moe(tokens.reshape(-1, D), rgw, rgb, rew, reb, w_gate, w_up, w_down).reshape(tokens.shape)
        xc = xc + cgt2 * y[:, :Lc]
        xl = xl + gt2 * y[:, Lc:]
    else:
        y = hier_moe(hl2.reshape(-1, D), rgw, rgb, rew, reb, w_gate, w_up, w_down).reshape(hl2.shape)
        xl = xl + gt2 * y
    return xl, xc


def setup_inputs(seed: int = 0) -> dict:
    key = jax.random.key(seed)
    ks = jax.random.split(key, 27)
    D = D_MODEL
    nrm = lambda k, shape, s: s * jax.random.normal(k, shape, F32)
    return {
        "x": nrm(ks[0], (BATCH, SEQ, D), 1.0),
        "c": nrm(ks[1], (BATCH, D), 1.0),
        "ctx": nrm(ks[2], (BATCH, CTX_LEN, D), 1.0),
        "c_ctx": nrm(ks[3], (D,), 1.0),
        "w_mod": nrm(ks[4], (DEPTH, D, N_MOD * D), 0.5 * D ** -0.5),
        "b_mod": nrm(ks[5], (DEPTH, N_MOD * D), 0.02),
        "norm1_g": 1.0 + nrm(ks[6], (DEPTH, D), 0.02),
        "norm2_g": 1.0 + nrm(ks[7], (DEPTH, D), 0.02),
        "w_in": nrm(ks[8], (DEPTH, D, IN_WIDTH), D ** -0.5),
        "w_out": nrm(ks[9], (DEPTH, MIX_WIDTH, D), MIX_WIDTH ** -0.5),
        "hg_lb_logits": nrm(ks[10], (DEPTH, HG_WIDTH), 0.1),
        "hg_norm_g": 1.0 + nrm(ks[11], (DEPTH, HEAD_DIM), 0.02),
        "q_norm_g": 1.0 + nrm(ks[12], (DEPTH, HEAD_DIM), 0.02),
        "k_norm_g": 1.0 + nrm(ks[13], (DEPTH, HEAD_DIM), 0.02),
        "lam_q1": nrm(ks[14], (DEPTH, DIFF_QK_DIM), 0.1),
        "lam_k1": nrm(ks[15], (DEPTH, DIFF_QK_DIM), 0.1),
        "lam_q2": nrm(ks[16], (DEPTH, DIFF_QK_DIM), 0.1),
        "lam_k2": nrm(ks[17], (DEPTH, DIFF_QK_DIM), 0.1),
        "diff_norm_g": 1.0 + nrm(ks[18], (DEPTH, HEAD_DIM), 0.02),
        "router_group_w": nrm(ks[19], (DEPTH, D, N_GROUPS), D ** -0.5),
        "router_group_b": nrm(ks[20], (DEPTH, N_GROUPS), 0.01),
        "router_expert_w": nrm(ks[21], (DEPTH, D, N_EXPERTS), D ** -0.5),
        "router_expert_b": nrm(ks[22], (DEPTH, N_EXPERTS), 0.01),
        "w_gate": nrm(ks[23], (DEPTH, N_EXPERTS, D, EXPERT_FF), D ** -0.5),
        "w_up": nrm(ks[24], (DEPTH, N_EXPERTS, D, EXPERT_FF), D ** -0.5),
        "w_down": nrm(ks[25], (DEPTH, N_EXPERTS, EXPERT_FF, D), EXPERT_FF ** -0.5),
        "final_norm_g": 1.0 + nrm(ks[26], (D,), 0.02),
    }


def reference(x, c, ctx, c_ctx, w_mod, b_mod, norm1_g, norm2_g, w_in, w_out, hg_lb_logits,
              hg_norm_g, q_norm_g, k_norm_g, lam_q1, lam_k1, lam_q2, lam_k2, diff_norm_g,
              router_group_w, router_group_b, router_expert_w, router_expert_b,
              w_gate, w_up, w_down, final_norm_g):
    n_tokens = x.shape[1]
    ropes = (axial_rope(n_tokens, HEAD_DIM), axial_rope(n_tokens, DIFF_QK_DIM))
    lb_all = jnp.cumsum(jax.nn.softmax(hg_lb_logits.astype(F32), axis=0), axis=0)
    lb_all = lb_all - lb_all[:1]
    xl, xc = x, ctx
    for l in range(DEPTH):
        xl, xc = layer(xl, xc, c, c_ctx, w_mod[l], b_mod[l], norm1_g[l], norm2_g[l], w_in[l], w_out[l],
                       lb_all[l], hg_norm_g[l], q_norm_g[l], k_norm_g[l],
                       lam_q1[l], lam_k1[l], lam_q2[l], lam_k2[l], diff_norm_g[l],
                       router_group_w[l], router_group_b[l], router_expert_w[l], router_expert_b[l],
                       w_gate[l], w_up[l], w_down[l], l, ropes, l < DEPTH - 1)
    return rms_norm(xl, final_norm_g)
```

```python
from contextlib import ExitStack
import numpy as np
import concourse.bass as bass
import concourse.mybir as mybir

F32 = mybir.dt.float32
BF16 = mybir.dt.bfloat16
ALU = mybir.AluOpType
AF = mybir.ActivationFunctionType
AX = mybir.AxisListType

ENGINES = ("pe", "act", "dve", "pool", "sp")
EPOCH = 12000


class Buf:
    __slots__ = ("name", "last_write", "reads", "last_dma", "excl")

    def __init__(self, name, excl=False):
        self.name = name
        self.excl = excl
        self.last_write = None
        self.reads = []
        self.last_dma = None


class Op:
    __slots__ = ("eng", "fns", "deps", "is_dma", "signal", "sem", "val", "sbuf", "gidx", "final")

    def __init__(self, eng, fns, is_dma):
        self.eng = eng
        self.fns = fns
        self.is_dma = is_dma
        self.deps = []
        self.signal = is_dma
        self.sem = None
        self.val = 0
        self.sbuf = None
        self.final = False


class Prog:
    def __init__(self, nc):
        self.nc = nc
        self.ops = {e: [] for e in ENGINES}
        self.all_ops = []
        self.stack = ExitStack()
        self.nbuf = 0

    def sbuf(self, name, shape, dtype):
        return self.stack.enter_context(self.nc.sbuf_tensor("s_" + name, list(shape), dtype))

    def psum(self, name, shape, dtype=F32):
        return self.stack.enter_context(self.nc.psum_tensor("p_" + name, list(shape), dtype))

    def buf(self, name=None, excl=False):
        self.nbuf += 1
        return Buf(name or f"b{self.nbuf}", excl)

    def _add(self, op, reads, writes):
        writes = list(writes) + [b for b in reads if b.excl and b not in writes]
        reads = [b for b in reads if not b.excl]
        deps = []
        for b in reads:
            if b.last_write is not None:
                deps.append(b.last_write)
        for b in writes:
            if b.last_write is not None:
                deps.append(b.last_write)
            deps.extend(b.reads)
        if op.is_dma and op.sbuf.last_dma is not None:
            deps.append(op.sbuf.last_dma)
        seen = set()
        for d in deps:
            if d is op or id(d) in seen:
                continue
            seen.add(id(d))
            if (not d.is_dma) and (not op.is_dma) and d.eng == op.eng and d.eng == "pe":
                continue
            op.deps.append(d)
            d.signal = True
        for b in reads:
            b.reads.append(op)
        for b in writes:
            b.last_write = op
            b.reads = []
        if op.is_dma:
            op.sbuf.last_dma = op
        op.gidx = len(self.all_ops)
        self.ops[op.eng].append(op)
        self.all_ops.append(op)
        return op

    def op(self, eng, fn, reads=(), writes=()):
        return self._add(Op(eng, [fn], False), list(reads), list(writes))

    def dma(self, eng, fns, sync, reads=(), writes=(), final=False):
        if callable(fns):
            fns = [fns]
        o = Op(eng, list(fns), True)
        o.sbuf = sync
        o.final = final
        return self._add(o, list(reads), list(writes))

    def emit(self):
        nc = self.nc
        st = self.stack
        nsem = [0]

        def new_sem(nm):
            nsem[0] += 1
            return st.enter_context(nc.semaphore(f"{nm}{nsem[0]}"))

        eng_sem, eng_cnt, buf_sem = {}, {}, {}
        for op in self.all_ops:
            if not op.signal:
                continue
            if op.is_dma:
                key = id(op.sbuf)
                if key not in buf_sem or buf_sem[key][1] >= EPOCH:
                    buf_sem[key] = [new_sem("d"), 0]
                buf_sem[key][1] += 16 * len(op.fns)
                op.sem, op.val = buf_sem[key][0], buf_sem[key][1]
            else:
                e = op.eng
                if e not in eng_sem or eng_cnt[e] >= EPOCH:
                    eng_sem[e] = new_sem("c")
                    eng_cnt[e] = 0
                eng_cnt[e] += 1
                op.sem, op.val = eng_sem[e], eng_cnt[e]
        self.n_sems = nsem[0]
        finals = [o for o in self.all_ops if o.final]
        engs = {"pe": nc.tensor, "act": nc.scalar, "dve": nc.vector, "pool": nc.gpsimd, "sp": nc.sync}
        prog = self

        with nc.Block() as block:
            def body(ename):
                eng = engs[ename]
                seen = {}
                for op in prog.ops[ename]:
                    for d in op.deps:
                        k = id(d.sem)
                        if seen.get(k, 0) >= d.val:
                            continue
                        eng.wait_ge(d.sem, d.val)
                        seen[k] = d.val
                    for fn in op.fns:
                        ins = fn(eng)
                        if op.is_dma:
                            ins.then_inc(op.sem, 16)
                    if op.signal and not op.is_dma:
                        ins.then_inc(op.sem, 1)
                if ename == "sp":
                    for d in finals:
                        eng.wait_ge(d.sem, d.val)

            @block.tensor
            def _(e):
                body("pe")

            @block.scalar
            def _(e):
                body("act")

            @block.vector
            def _(e):
                body("dve")

            @block.gpsimd
            def _(e):
                body("pool")

            @block.sync
            def _(e):
                body("sp")

    def close(self):
        self.stack.close()


from concourse.bass_utils import run_bass_kernel_spmd
import ml_dtypes

D = 2048
NCH = 16
EPS = 1e-6


class Ring:
    def __init__(self, P, name, n, shape, dtype, psum=False):
        self.items = []
        for i in range(n):
            t = (P.psum if psum else P.sbuf)(f"{name}{i}", shape, dtype)
            self.items.append((t, P.buf(f"{name}{i}", excl=psum)))
        self.i = 0

    @staticmethod
    def of(items):
        r = Ring.__new__(Ring)
        r.items = list(items)
        r.i = 0
        return r

    def next(self):
        it = self.items[self.i % len(self.items)]
        self.i += 1
        return it


def new_nc():
    return bass.Bass("TRN2", target_bir_lowering=False)


def din(nc, name, shape, dt=F32):
    return nc.dram_tensor(name, list(shape), dt, kind="ExternalInput").ap()


def dout(nc, name, shape, dt=F32):
    return nc.dram_tensor(name, list(shape), dt, kind="ExternalOutput").ap()


def blocks(T, NL, bs=512):
    out = []
    c = 0
    while c < NL:
        n = min(bs, NL - c)
        out.append((c, n, 0))
        c += n
    while c < T:
        n = min(bs, T - c)
        out.append((c, n, 1))
        c += n
    return out


def norm_mod(P, xT, Bx, a, Ba, b, Bb, hT, Bh, T, NL, ones, Bones, hF=None, BhF=None):
    sqr = Ring(P, "nm_sq", 2, [128, 512], BF16)
    tmpr = Ring(P, "nm_tmp", 2, [128, 512], F32)
    ssr = Ring(P, "nm_ss", 1, [128, 512], F32, psum=True)
    rsr = Ring(P, "nm_rs", 2, [128, 512], F32)
    def do_block(c0, n, j):
        ss, Bss = ssr.next()
        for c in range(NCH):
            sq, Bsq = sqr.next()
            P.op("act", lambda e, sq=sq, c=c: e.activation(out=sq[:, :n], in_=xT[:, c, c0:c0 + n], func=AF.Square),
                 reads=[Bx], writes=[Bsq])
            P.op("pe", lambda e, sq=sq, c=c, ss=ss: e.matmul(ss[:, :n], lhsT=ones[:], rhs=sq[:, :n], start=(c == 0), stop=(c == NCH - 1)),
                 reads=[Bsq, Bones], writes=[Bss])
        rs, Brs = rsr.next()
        P.op("dve", lambda e, rs=rs, ss=ss: e.tensor_scalar(out=rs[:, :n], in0=ss[:, :n], scalar1=1.0 / D, scalar2=EPS, op0=ALU.mult, op1=ALU.add),
             reads=[Bss], writes=[Brs])
        P.op("act", lambda e, rs=rs: e.activation(out=rs[:, :n], in_=rs[:, :n], func=AF.Sqrt), reads=[Brs], writes=[Brs])
        P.op("dve", lambda e, rs=rs: e.reciprocal(out=rs[:, :n], in_=rs[:, :n]), reads=[Brs], writes=[Brs])
        for c in range(NCH):
            tmp, Btmp = tmpr.next()
            P.op("dve", lambda e, tmp=tmp, c=c, rs=rs: e.tensor_tensor(out=tmp[:, :n], in0=xT[:, c, c0:c0 + n], in1=rs[:, :n], op=ALU.mult),
                 reads=[Bx, Brs], writes=[Btmp])
            P.op("act", lambda e, tmp=tmp, c=c: e.activation(out=hT[:, c, c0:c0 + n], in_=tmp[:, :n], func=AF.Identity,
                                                             scale=a[:, c, j:j + 1], bias=b[:, c, j:j + 1]),
                 reads=[Btmp, Ba, Bb], writes=[Bh])
            if hF is not None:
                P.op("pool", lambda e, tmp=tmp, c=c: e.tensor_scalar(out=hF[:, c, c0:c0 + n], in0=tmp[:, :n], scalar1=a[:, c, j:j + 1],
                                                                    scalar2=b[:, c, j:j + 1], op0=ALU.mult, op1=ALU.add),
                     reads=[Btmp, Ba, Bb], writes=[BhF])

    for (c0_, n_, j_) in blocks(T, NL):
        do_block(c0_, n_, j_)


def gemm_tok(P, hT, Bh, w_d, ncols, out_d, T, kch=NCH):
    wv = w_d.rearrange("(c p) n -> p c n", p=128)
    wr = Ring(P, "g_w", 2, [128, kch, 512], BF16)
    psr = Ring(P, "g_ps", 2, [128, 512], F32, psum=True)
    osr = Ring(P, "g_os", 3, [128, 512], F32)
    Bout = P.buf("g_out")
    k = 0
    for nb in range(ncols // 512):
        wb, Bw = wr.next()
        P.dma("pool", lambda e, wb=wb, nb=nb: e.dma_start(out=wb[:], in_=wv[:, :, nb * 512:(nb + 1) * 512]), Bw, writes=[Bw])
        for m0 in range(0, T, 128):
            mt = min(128, T - m0)
            ps, Bps = psr.next()
            for c in range(kch):
                P.op("pe", lambda e, ps=ps, c=c, wb=wb, m0=m0, mt=mt: e.matmul(ps[:mt, :], lhsT=hT[:, c, m0:m0 + mt], rhs=wb[:, c, :],
                                                                            start=(c == 0), stop=(c == kch - 1)),
                     reads=[Bh, Bw], writes=[Bps])
            os_, Bos = osr.next()
            if k % 2 == 0:
                P.op("act", lambda e, os_=os_, ps=ps, mt=mt: e.activation(out=os_[:mt, :], in_=ps[:mt, :], func=AF.Identity), reads=[Bps], writes=[Bos])
            else:
                P.op("dve", lambda e, os_=os_, ps=ps, mt=mt: e.tensor_copy(out=os_[:mt, :], in_=ps[:mt, :]), reads=[Bps], writes=[Bos])
            k += 1
            P.dma("sp", lambda e, os_=os_, m0=m0, mt=mt, nb=nb: e.dma_start(out=out_d[m0:m0 + mt, nb * 512:(nb + 1) * 512], in_=os_[:mt, :]),
                  Bos, reads=[Bos], writes=[Bout], final=True)


def load_consts(P, T):
    ones = P.sbuf("ones", [128, 128], BF16)
    Bones = P.buf("ones")
    P.op("dve", lambda e: e.memset(ones[:], 1.0), writes=[Bones])
    return ones, Bones


def mod_ab(P, mod, Bmod, g, Bg, sh_k, sc_k, name):
    a = P.sbuf(name + "_a", [128, NCH, 2], F32)
    Ba = P.buf(name + "_a")
    for j in range(2):
        P.op("dve", lambda e, j=j: e.scalar_tensor_tensor(out=a[:, :, j], in0=mod[:, sc_k * 16:(sc_k + 1) * 16, j], scalar=1.0, in1=g[:, :],
                                                          op0=ALU.add, op1=ALU.mult), reads=[Bmod, Bg], writes=[Ba])
    return a, Ba, mod[:, sh_k * 16:(sh_k + 1) * 16, :], Bmod


def build_M():
    nc = new_nc()
    cc_d = din(nc, "cc", [128, NCH, 2])
    wm_d = din(nc, "wm", [2, 2048, 1536])
    bm_d = din(nc, "bm", [2, 128, 12])
    mo_d = dout(nc, "mo", [2, 128, 12, 2])
    P = Prog(nc)
    cc = P.sbuf("cc", [128, NCH, 2], F32); Bcc = P.buf()
    sg = P.sbuf("sg", [128, NCH, 2], F32); Bsg = P.buf()
    bm = P.sbuf("bm", [128, 2, 12], F32); Bbm = P.buf()
    mo = P.sbuf("mo", [128, 2, 12, 2], F32); Bmo = P.buf()
    P.dma("sp", lambda e: e.dma_start(out=cc[:], in_=cc_d[:, :, :]), Bcc, writes=[Bcc])
    P.dma("sp", [lambda e, l=l: e.dma_start(out=bm[:, l, :], in_=bm_d[l, :, :]) for l in range(2)], Bbm, writes=[Bbm])
    P.op("act", lambda e: e.activation(out=sg[:], in_=cc[:], func=AF.Sigmoid), reads=[Bcc], writes=[Bsg])
    P.op("dve", lambda e: e.tensor_tensor(out=sg[:], in0=sg[:], in1=cc[:], op=ALU.mult), reads=[Bcc, Bsg], writes=[Bsg])
    wt = P.sbuf("wmt", [128, NCH, 1536], F32); Bw = P.buf("wmt")
    psr = Ring(P, "mps", 2, [128, 256, 2], F32, psum=True)
    Bout = P.buf()
    for l in range(2):
        wv = wm_d[l].rearrange("(c p) n -> p c n", p=128)
        ps, Bps = psr.next()
        P.dma("sp", [lambda e, kg=kg, wv=wv: e.dma_start(out=wt[:, kg * 4:(kg + 1) * 4, :], in_=wv[:, kg * 4:(kg + 1) * 4, :]) for kg in range(4)], Bw, writes=[Bw])
        for j in range(12):
            for k in range(NCH):
                P.op("pe", lambda e, ps=ps, j=j, k=k: e.matmul(ps[:, j, :], lhsT=wt[:, k, j * 128:(j + 1) * 128], rhs=sg[:, k, :],
                                                            start=(k == 0), stop=(k == 15)),
                     reads=[Bw, Bsg], writes=[Bps])
        for j in range(12):
            P.op("dve", lambda e, ps=ps, j=j, l=l: e.tensor_scalar(out=mo[:, l, j, :], in0=ps[:, j, :], scalar1=bm[:, l, j:j + 1], scalar2=None, op0=ALU.add),
                 reads=[Bps, Bbm], writes=[Bmo])
    P.dma("sp", [lambda e, l=l: e.dma_start(out=mo_d[l], in_=mo[:, l, :, :]) for l in range(2)], Bmo, reads=[Bmo], writes=[Bout], final=True)
    P.emit(); P.close()
    return nc


def build_A(T, NL):
    nc = new_nc()
    xT_d = din(nc, "xT", [128, NCH, T])
    mod_d = din(nc, "mod", [128, 96, 2])
    g_d = din(nc, "g1", [128, NCH])
    w_d = din(nc, "w", [2048, 6144])
    p_d = dout(nc, "p", [T, 6144])
    P = Prog(nc)
    xT = P.sbuf("xT", [128, NCH, T], F32); Bx = P.buf("xT")
    mod = P.sbuf("mod", [128, 96, 2], F32); Bmod = P.buf("mod")
    g = P.sbuf("g", [128, NCH], F32); Bg = P.buf("g")
    hT = P.sbuf("hT", [128, NCH, T], BF16); Bh = P.buf("hT")
    P.dma("sp", [lambda e, q=q: e.dma_start(out=xT[:, q * 4:(q + 1) * 4, :], in_=xT_d[:, q * 4:(q + 1) * 4, :]) for q in range(4)], Bx, writes=[Bx])
    P.dma("sp", lambda e: e.dma_start(out=mod[:], in_=mod_d[:, :, :]), Bmod, writes=[Bmod])
    P.dma("sp", lambda e: e.dma_start(out=g[:], in_=g_d[:, :]), Bg, writes=[Bg])
    ones, Bones = load_consts(P, T)
    a, Ba, b, Bb = mod_ab(P, mod, Bmod, g, Bg, 0, 1, "m1")
    norm_mod(P, xT, Bx, a, Ba, b, Bb, hT, Bh, T, NL, ones, Bones)
    gemm_tok(P, hT, Bh, w_d, 6144, p_d, T)
    P.emit(); P.close()
    return nc


def groups(n, g):
    out = []
    i = 0
    while i < n:
        out.append((i, min(g, n - i)))
        i += g
    return out


def prep_qk(P, name, src_d, N, dstT, BdstT, ident, Bident, tpr, gain=None, Bgain=None, cos_d=None, sin_d=None):
    NT = N // 128
    srcv = src_d.rearrange("(t p) d -> p t d", p=128)
    GR = 6
    if not hasattr(P, "_prep"):
        P._prep = dict(
            xr=Ring(P, "pp_x", 2, [128, GR, 128], F32), cr=Ring(P, "pp_c", 2, [128, GR, 64], F32), sr=Ring(P, "pp_s", 2, [128, GR, 64], F32),
            ssr=Ring(P, "pp_ss", 3, [128, 1], F32), jr=Ring(P, "pp_j", 2, [128, 128], F32), xnr=Ring(P, "pp_xn", 3, [128, 128], F32),
            t1r=Ring(P, "pp_t1", 3, [128, 64], F32), t2r=Ring(P, "pp_t2", 3, [128, 64], F32), yr=Ring(P, "pp_y", 3, [128, 128], BF16))
    R_ = P._prep
    xr, cr, sr, ssr, jr, xnr, t1r, t2r, yr = (R_[k] for k in ("xr", "cr", "sr", "ssr", "jr", "xnr", "t1r", "t2r", "yr"))
    if cos_d is not None:
        cosv = cos_d.rearrange("(t p) d -> p t d", p=128)
        sinv = sin_d.rearrange("(t p) d -> p t d", p=128)

    def do_tile(x, Bx, cs, Bcs, sn, Bsn, gi, t):
        xin = x[:, gi, :]
        cur, Bcur = xin, Bx
        if gain is not None:
            ss, Bss = ssr.next()
            junk, Bj = jr.next()
            P.op("act", lambda e: e.activation(out=junk[:], in_=xin, func=AF.Square), reads=[Bx], writes=[Bj])
            P.op("dve", lambda e: e.reduce_sum(out=ss[:], in_=junk[:], axis=AX.X), reads=[Bj], writes=[Bss])
            P.op("dve", lambda e: e.tensor_scalar(out=ss[:], in0=ss[:], scalar1=1.0 / 128, scalar2=EPS, op0=ALU.mult, op1=ALU.add), reads=[Bss], writes=[Bss])
            P.op("act", lambda e: e.activation(out=ss[:], in_=ss[:], func=AF.Sqrt), reads=[Bss], writes=[Bss])
            P.op("dve", lambda e: e.reciprocal(out=ss[:], in_=ss[:]), reads=[Bss], writes=[Bss])
            xn, Bxn = xnr.next()
            P.op("dve", lambda e: e.scalar_tensor_tensor(out=xn[:], in0=xin, scalar=ss[:, 0:1], in1=gain[:], op0=ALU.mult, op1=ALU.mult),
                 reads=[Bx, Bss, Bgain], writes=[Bxn])
            cur, Bcur = xn[:], Bxn
        y, By = yr.next()
        if cos_d is not None:
            cur3 = cur.rearrange("p (i two) -> p i two", two=2)
            x0, x1 = cur3[:, :, 0], cur3[:, :, 1]
            y3 = y[:].rearrange("p (i two) -> p i two", two=2)
            t1, Bt1 = t1r.next()
            t2, Bt2 = t2r.next()
            c_, s_ = cs[:, gi, :], sn[:, gi, :]
            P.op("dve", lambda e: e.tensor_tensor(out=t1[:], in0=x0, in1=c_, op=ALU.mult), reads=[Bcur, Bcs], writes=[Bt1])
            P.op("pool", lambda e: e.tensor_tensor(out=t2[:], in0=x1, in1=s_, op=ALU.mult), reads=[Bcur, Bsn], writes=[Bt2])
            P.op("dve", lambda e: e.tensor_tensor(out=y3[:, :, 0], in0=t1[:], in1=t2[:], op=ALU.subtract), reads=[Bt1, Bt2], writes=[By])
            t3, Bt3 = t1r.next()
            t4, Bt4 = t2r.next()
            P.op("pool", lambda e: e.tensor_tensor(out=t3[:], in0=x0, in1=s_, op=ALU.mult), reads=[Bcur, Bsn], writes=[Bt3])
            P.op("dve", lambda e: e.tensor_tensor(out=t4[:], in0=x1, in1=c_, op=ALU.mult), reads=[Bcur, Bcs], writes=[Bt4])
            P.op("pool", lambda e: e.tensor_tensor(out=y3[:, :, 1], in0=t3[:], in1=t4[:], op=ALU.add), reads=[Bt3, Bt4, By], writes=[By])
        else:
            P.op("act", lambda e: e.activation(out=y[:], in_=cur, func=AF.Identity), reads=[Bcur], writes=[By])
        tp, Btp = tpr.next()
        P.op("pe", lambda e: e.transpose(tp[:], y[:], ident[:]), reads=[By, Bident], writes=[Btp])
        P.op("act", lambda e: e.activation(out=dstT[:, t * 128:(t + 1) * 128], in_=tp[:], func=AF.Identity), reads=[Btp], writes=[BdstT])

    def do_group(g0, gn):
        x, Bx = xr.next()
        P.dma("sp", lambda e: e.dma_start(out=x[:, :gn, :], in_=srcv[:, g0:g0 + gn, :]), Bx, writes=[Bx])
        cs = sn = Bcs = Bsn = None
        if cos_d is not None:
            cs, Bcs = cr.next()
            sn, Bsn = sr.next()
            P.dma("sp", lambda e: e.dma_start(out=cs[:, :gn, :], in_=cosv[:, g0:g0 + gn, :]), Bcs, writes=[Bcs])
            P.dma("sp", lambda e: e.dma_start(out=sn[:, :gn, :], in_=sinv[:, g0:g0 + gn, :]), Bsn, writes=[Bsn])
        for gi in range(gn):
            do_tile(x, Bx, cs, Bcs, sn, Bsn, gi, g0 + gi)

    for (g0, gn) in groups(NT, GR):
        do_group(g0, gn)


class AttnRes:
    pass


def attn_setup(P, banks):
    R = AttnRes()
    R.stp = Ring.of(banks[0:2])
    R.pt = Ring(P, "at_pt", 3, [128, 512], BF16)
    R.o = [banks[2], banks[3]]
    R.s = [banks[4], banks[5]]
    R.rec = Ring(P, "at_rec", 2, [128, 512], F32)
    return R


def attn_head(P, R, s, qT, BqT, kT, BkT, v, Bv, ones, Bones, q0, qn, kt0, kt1, pl, ph, scale):
    o, Bo = R.o[s]
    sm, Bsm = R.s[s]

    def do_kt(kt):
        st, Bst = R.stp.next()
        P.op("pe", lambda e: e.matmul(st[:, :qn], lhsT=kT[pl:ph, kt * 128:(kt + 1) * 128], rhs=qT[pl:ph, q0:q0 + qn], start=True, stop=True),
             reads=[BkT, BqT], writes=[Bst])
        pt, Bpt = R.pt.next()
        P.op("act", lambda e: e.activation(out=pt[:, :qn], in_=st[:, :qn], func=AF.Exp, scale=scale), reads=[Bst], writes=[Bpt])
        P.op("pe", lambda e: e.matmul(o[:, :qn], lhsT=v[:, kt, :], rhs=pt[:, :qn], start=(kt == kt0), stop=(kt == kt1 - 1)),
             reads=[Bv, Bpt], writes=[Bo])
        P.op("pe", lambda e: e.matmul(sm[:, :qn], lhsT=ones[:], rhs=pt[:, :qn], start=(kt == kt0), stop=(kt == kt1 - 1)),
             reads=[Bones, Bpt], writes=[Bsm])

    for kt in range(kt0, kt1):
        do_kt(kt)


def build_B(layer, G=8448, NC_CTX=256, lam_init=0.2, parts=("gqa", "diff", "hgrn")):
    GQ = 128 + 4096
    NKT = G // 128
    nc = new_nc()
    ident_d = din(nc, "ident", [128, 128])
    gq_d = din(nc, "gq", [G, 128]); gk_d = din(nc, "gk", [G, 128]); gv_d = din(nc, "gv", [G, 128])
    qn_d = din(nc, "qn", [128, 128]); kn_d = din(nc, "kn", [128, 128])
    cosa_d = din(nc, "cosa", [G, 64]); sina_d = din(nc, "sina", [G, 64])
    dq_d = din(nc, "dq", [GQ, 128]); dk_d = din(nc, "dk", [G, 128]); dv_d = din(nc, "dv", [G, 128])
    cosdq_d = din(nc, "cosdq", [GQ, 64]); sindq_d = din(nc, "sindq", [GQ, 64])
    cosdk_d = din(nc, "cosdk", [G, 64]); sindk_d = din(nc, "sindk", [G, 64])
    lamv_d = din(nc, "lamv", [128, 4, 64])
    dn_d = din(nc, "dn", [128, 1])
    hq_d = din(nc, "hq", [G, 128]); hf_d = din(nc, "hf", [G, 128]); hv_d = din(nc, "hv", [G, 128])
    lbl_d = din(nc, "lbl", [128, 2, 128])
    tri_d = din(nc, "tri", [128, 128]); blk_d = din(nc, "blk", [128, 128]); cm_d = din(nc, "cm", [128, 4])
    attT_d = dout(nc, "attT", [128, G])
    diffT_d = dout(nc, "diffT", [128, GQ])
    ohT_d = dout(nc, "ohT", [128, G])
    P = Prog(nc)
    ones, Bones = load_consts(P, 0)
    ident = P.sbuf("ident", [128, 128], BF16); Bident = P.buf()
    P.dma("pool", lambda e: e.dma_start(out=ident[:], in_=ident_d[:, :]), Bident, writes=[Bident])
    Bfin = P.buf("fin")
    banks = [(P.psum(f"bank{i}", [128, 512]), P.buf(f"bank{i}", excl=True)) for i in range(6)]
    bank7 = (P.psum("bank7", [128, 512]), P.buf("bank7", excl=True))
    tp6 = P.psum("bank6", [128, 8, 128], BF16)
    Btp6 = P.buf("bank6", excl=True)
    tpr = Ring.of([(tp6[:, i, :], Btp6) for i in range(2)])
    R = attn_setup(P, banks)
    ostage = Ring(P, "ostage", 2, [128, 512], F32)

    qn = P.sbuf("qn", [128, 128], F32); Bqn = P.buf()
    kn = P.sbuf("kn", [128, 128], F32); Bkn = P.buf()
    P.dma("sp", lambda e: e.dma_start(out=qn[:], in_=qn_d[:, :]), Bqn, writes=[Bqn])
    P.dma("sp", lambda e: e.dma_start(out=kn[:], in_=kn_d[:, :]), Bkn, writes=[Bkn])
    qT = P.sbuf("qT", [128, G], BF16); BqT = P.buf()
    kT = P.sbuf("kT", [128, G], BF16); BkT = P.buf()
    vv = P.sbuf("vv", [128, NKT, 128], BF16); Bvv = P.buf()
    P.dma("pool", [lambda e, h=h: e.dma_start(out=vv[:, h * 33:(h + 1) * 33, :], in_=gv_d.rearrange("(t p) d -> p t d", p=128)[:, h * 33:(h + 1) * 33, :]) for h in range(2)],
          Bvv, writes=[Bvv])
    prep_qk(P, "pq", gq_d, G, qT, BqT, ident, Bident, tpr, qn, Bqn, cosa_d, sina_d)
    prep_qk(P, "pk", gk_d, G, kT, BkT, ident, Bident, tpr, kn, Bkn, cosa_d, sina_d)

    if "dbg" in parts:
        dbgq_d = dout(nc, "dbgq", [128, G], BF16)
        dbgk_d = dout(nc, "dbgk", [128, G], BF16)
        P.dma("sp", lambda e: e.dma_start(out=dbgq_d[:, :], in_=qT[:]), BqT, reads=[BqT], writes=[Bfin], final=True)
        P.dma("sp", lambda e: e.dma_start(out=dbgk_d[:, :], in_=kT[:]), BkT, reads=[BkT], writes=[Bfin], final=True)

    def gqa_block(q0, qn_, kt0, kt1):
        attn_head(P, R, 0, qT, BqT, kT, BkT, vv, Bvv, ones, Bones, q0, qn_, kt0, kt1, 0, 128, 128 ** -0.5)
        o, Bo = R.o[0]; sm, Bsm = R.s[0]
        rec, Brec = R.rec.next()
        P.op("dve", lambda e: e.reciprocal(out=rec[:, :qn_], in_=sm[:, :qn_]), reads=[Bsm], writes=[Brec])
        os_, Bos = ostage.next()
        P.op("dve", lambda e: e.tensor_tensor(out=os_[:, :qn_], in0=o[:, :qn_], in1=rec[:, :qn_], op=ALU.mult), reads=[Bo, Brec], writes=[Bos])
        P.dma("sp", lambda e: e.dma_start(out=attT_d[:, q0:q0 + qn_], in_=os_[:, :qn_]), Bos, reads=[Bos], writes=[Bfin], final=True)

    if "gqa" in parts:
        gqa_block(0, NC_CTX, 0, NC_CTX // 128)
        for b in range((G - NC_CTX) // 512):
            gqa_block(NC_CTX + b * 512, 512, 0, NKT)

    lamv = P.sbuf("lamv", [128, 4, 64], F32); Blamv = P.buf()
    P.dma("sp", lambda e: e.dma_start(out=lamv[:], in_=lamv_d[:, :, :]), Blamv, writes=[Blamv])
    dn = P.sbuf("dn", [128, 1], F32); Bdn = P.buf()
    P.dma("sp", lambda e: e.dma_start(out=dn[:], in_=dn_d[:, :]), Bdn, writes=[Bdn])
    lpr = P.sbuf("lpr", [128, 2, 64], F32); Blpr = P.buf()
    lsum = P.sbuf("lsum", [128, 2], F32); Blsum = P.buf()
    nlam = P.sbuf("nlam", [128, 1], F32); Bnlam = P.buf()
    P.op("dve", lambda e: e.tensor_tensor(out=lpr[:, 0, :], in0=lamv[:, 0, :], in1=lamv[:, 1, :], op=ALU.mult), reads=[Blamv], writes=[Blpr])
    P.op("dve", lambda e: e.tensor_tensor(out=lpr[:, 1, :], in0=lamv[:, 2, :], in1=lamv[:, 3, :], op=ALU.mult), reads=[Blamv, Blpr], writes=[Blpr])
    P.op("dve", lambda e: e.reduce_sum(out=lsum[:], in_=lpr[:], axis=AX.X), reads=[Blpr], writes=[Blsum])
    P.op("act", lambda e: e.activation(out=lsum[:], in_=lsum[:], func=AF.Exp), reads=[Blsum], writes=[Blsum])
    P.op("dve", lambda e: e.scalar_tensor_tensor(out=nlam[:], in0=lsum[:, 1:2], scalar=-lam_init, in1=lsum[:, 0:1], op0=ALU.add, op1=ALU.subtract),
         reads=[Blsum], writes=[Bnlam])
    P.op("dve", lambda e: e.tensor_scalar(out=dn[:], in0=dn[:], scalar1=1.0 - lam_init, scalar2=None, op0=ALU.mult), reads=[Bdn], writes=[Bdn])
    dqT, BdqT = qT, BqT
    dkT, BdkT = kT, BkT
    dvv, Bdvv = vv, Bvv
    P.dma("pool", [lambda e, h=h: e.dma_start(out=dvv[:, h * 33:(h + 1) * 33, :], in_=dv_d.rearrange("(t p) d -> p t d", p=128)[:, h * 33:(h + 1) * 33, :]) for h in range(2)],
          Bdvv, writes=[Bdvv])
    prep_qk(P, "dq", dq_d, GQ, dqT, BdqT, ident, Bident, tpr, None, None, cosdq_d, sindq_d)
    prep_qk(P, "dk", dk_d, G, dkT, BdkT, ident, Bident, tpr, None, None, cosdk_d, sindk_d)
    dt0 = Ring(P, "dt0", 2, [128, 512], F32)
    dt1 = Ring(P, "dt1", 2, [128, 512], F32)
    dsq = Ring(P, "dsq", 2, [128, 512], BF16)
    dss, Bdss = bank7
    drs = Ring(P, "drs", 2, [128, 512], F32)

    def diff_block(q0, qn_, kt0, kt1):
        for s in range(2):
            attn_head(P, R, s, dqT, BdqT, dkT, BdkT, dvv, Bdvv, ones, Bones, q0, qn_, kt0, kt1, 64 * s, 64 * s + 64, 64 ** -0.5)
        t = []
        for s, ring in ((0, dt0), (1, dt1)):
            o, Bo = R.o[s]; sm, Bsm = R.s[s]
            rec, Brec = R.rec.next()
            P.op("dve", lambda e, rec=rec, sm=sm: e.reciprocal(out=rec[:, :qn_], in_=sm[:, :qn_]), reads=[Bsm], writes=[Brec])
            tt, Btt = ring.next()
            P.op("dve", lambda e, tt=tt, o=o, rec=rec: e.tensor_tensor(out=tt[:, :qn_], in0=o[:, :qn_], in1=rec[:, :qn_], op=ALU.mult), reads=[Bo, Brec], writes=[Btt])
            t.append((tt, Btt))
        (t0, Bt0), (t1, Bt1) = t
        P.op("dve", lambda e: e.scalar_tensor_tensor(out=t0[:, :qn_], in0=t1[:, :qn_], scalar=nlam[:, 0:1], in1=t0[:, :qn_], op0=ALU.mult, op1=ALU.add),
             reads=[Bt1, Bt0, Bnlam], writes=[Bt0])
        sq, Bsq = dsq.next()
        P.op("act", lambda e: e.activation(out=sq[:, :qn_], in_=t0[:, :qn_], func=AF.Square), reads=[Bt0], writes=[Bsq])
        P.op("pe", lambda e: e.matmul(dss[:, :qn_], lhsT=ones[:], rhs=sq[:, :qn_], start=True, stop=True), reads=[Bsq, Bones], writes=[Bdss])
        rs, Brs = drs.next()
        P.op("dve", lambda e: e.tensor_scalar(out=rs[:, :qn_], in0=dss[:, :qn_], scalar1=1.0 / 128, scalar2=EPS, op0=ALU.mult, op1=ALU.add), reads=[Bdss], writes=[Brs])
        P.op("act", lambda e: e.activation(out=rs[:, :qn_], in_=rs[:, :qn_], func=AF.Sqrt), reads=[Brs], writes=[Brs])
        P.op("dve", lambda e: e.reciprocal(out=rs[:, :qn_], in_=rs[:, :qn_]), reads=[Brs], writes=[Brs])
        os_, Bos = ostage.next()
        P.op("dve", lambda e: e.scalar_tensor_tensor(out=os_[:, :qn_], in0=t0[:, :qn_], scalar=dn[:, 0:1], in1=rs[:, :qn_], op0=ALU.mult, op1=ALU.mult),
             reads=[Bt0, Bdn, Brs], writes=[Bos])
        P.dma("sp", lambda e: e.dma_start(out=diffT_d[:, q0:q0 + qn_], in_=os_[:, :qn_]), Bos, reads=[Bos], writes=[Bfin], final=True)

    if "diff" in parts:
        diff_block(0, 128, 0, NC_CTX // 128)
        for b in range(4096 // 512):
            diff_block(128 + b * 512, 512, 0, NKT)

    lbl = P.sbuf("lbl", [128, 2, 128], F32); Blbl = P.buf()
    P.dma("sp", lambda e: e.dma_start(out=lbl[:], in_=lbl_d[:, :, :]), Blbl, writes=[Blbl])
    lb = P.sbuf("lb", [128, 128], F32); Blb = P.buf()
    oml = P.sbuf("oml", [128, 128], F32); Boml = P.buf()
    P.op("dve", lambda e: e.tensor_tensor(out=lb[:], in0=lbl[:, 1, :], in1=lbl[:, 0, :], op=ALU.subtract), reads=[Blbl], writes=[Blb])
    P.op("act", lambda e: e.activation(out=lb[:], in_=lb[:], func=AF.Sigmoid), reads=[Blb], writes=[Blb])
    P.op("dve", lambda e: e.tensor_scalar(out=lb[:], in0=lb[:], scalar1=float(layer), scalar2=None, op0=ALU.mult), reads=[Blb], writes=[Blb])
    P.op("dve", lambda e: e.tensor_scalar(out=oml[:], in0=lb[:], scalar1=-1.0, scalar2=1.0, op0=ALU.mult, op1=ALU.add), reads=[Blb], writes=[Boml])
    tri = P.sbuf("tri", [128, 128], F32); Btri = P.buf()
    blk = P.sbuf("blk", [128, 128], F32); Bblk = P.buf()
    cm = P.sbuf("cm", [128, 4], F32); Bcm = P.buf()
    P.dma("sp", lambda e: e.dma_start(out=tri[:], in_=tri_d[:, :]), Btri, writes=[Btri])
    P.dma("sp", lambda e: e.dma_start(out=blk[:], in_=blk_d[:, :]), Bblk, writes=[Bblk])
    P.dma("sp", lambda e: e.dma_start(out=cm[:], in_=cm_d[:, :]), Bcm, writes=[Bcm])
    S = P.sbuf("S", [128, 128], F32); BS = P.buf()
    P.op("dve", lambda e: e.memset(S[:], 0.0), writes=[BS])
    Sbr = Ring(P, "Sb", 2, [128, 128], BF16)
    Sb, BSb = Sbr.next()
    P.op("dve", lambda e: e.memset(Sb[:], 0.0), writes=[BSb])
    GR = 6
    hqv = hq_d.rearrange("(t p) d -> p t d", p=128)
    hfv = hf_d.rearrange("(t p) d -> p t d", p=128)
    hvv_ = hv_d.rearrange("(t p) d -> p t d", p=128)
    hqr = Ring(P, "hq", 2, [128, GR, 128], F32)
    hfr = Ring(P, "hf", 2, [128, GR, 128], F32)
    hvr = Ring(P, "hv", 2, [128, GR, 128], BF16)
    f_r = Ring(P, "h_f", 2, [128, 128], F32)
    lf_r = Ring(P, "h_lf", 2, [128, 128], F32)
    kk_r = Ring(P, "h_kk", 2, [128, 128], F32)
    bps_r = Ring.of([(banks[4][0][:, 0:128], banks[4][1])])
    bt_r = Ring.of([(banks[5][0][:, 0:128], banks[5][1])])
    bsb_r = Ring(P, "h_bsb", 2, [128, 128], F32)
    e_r = Ring(P, "h_e", 4, [128, 128], F32)
    qt_r = Ring(P, "h_qt", 2, [128, 128], BF16)
    kt_r = Ring(P, "h_kt", 2, [128, 128], BF16)
    ke_r = Ring(P, "h_ke", 2, [128, 128], BF16)
    kem_r = Ring(P, "h_kem", 4, [128, 128], BF16)
    tp_r = tpr
    qtT_r = Ring(P, "h_qtT", 2, [128, 128], BF16)
    ktT_r = Ring(P, "h_ktT", 2, [128, 128], BF16)
    at_r = Ring.of([(banks[0][0][:, 0:128], banks[0][1])])
    atm_r = Ring(P, "h_atm", 2, [128, 128], BF16)
    dec_ps_r = Ring.of([(banks[1][0][:, 0:4], banks[1][1])])
    dec_r = Ring(P, "h_dec", 2, [128, 4], F32)
    u_r = Ring.of([(banks[3][0][:, 0:128], banks[3][1])])
    oT_r = Ring.of([(banks[2][0][:, 0:128], banks[2][1])])
    hos_r = Ring(P, "h_os", 2, [128, 512], F32)
    state = {"Sb": Sb, "BSb": BSb, "os": None}

    def h_tile(xq, Bxq, xf, Bxf, xv, Bxv, gi, t):
        z = xf[:, gi, :]
        f, Bf = f_r.next()
        P.op("act", lambda e: e.activation(out=f[:], in_=z, func=AF.Sigmoid), reads=[Bxf], writes=[Bf])
        P.op("dve", lambda e: e.tensor_tensor(out=f[:], in0=f[:], in1=oml[:], op=ALU.mult), reads=[Bf, Boml], writes=[Bf])
        P.op("dve", lambda e: e.tensor_tensor(out=f[:], in0=f[:], in1=lb[:], op=ALU.add), reads=[Bf, Blb], writes=[Bf])
        lf, Blf = lf_r.next()
        P.op("act", lambda e: e.activation(out=lf[:], in_=f[:], func=AF.Ln), reads=[Bf], writes=[Blf])
        kk, Bkk = kk_r.next()
        P.op("pool", lambda e: e.tensor_scalar(out=kk[:], in0=f[:], scalar1=-1.0, scalar2=1.0, op0=ALU.mult, op1=ALU.add), reads=[Bf], writes=[Bkk])
        bps, Bbps = bps_r.next()
        P.op("pe", lambda e: e.matmul(bps[:], lhsT=tri[:], rhs=lf[:], start=True, stop=True), reads=[Btri, Blf], writes=[Bbps])
        bt, Bbt = bt_r.next()
        P.op("pe", lambda e: e.matmul(bt[:], lhsT=blk[:], rhs=lf[:], start=True, stop=True), reads=[Bblk, Blf], writes=[Bbt])
        dps, Bdps = dec_ps_r.next()
        P.op("pe", lambda e: e.matmul(dps[:], lhsT=lf[:], rhs=cm[:], start=True, stop=True), reads=[Blf, Bcm], writes=[Bdps])
        dec, Bdec = dec_r.next()
        P.op("act", lambda e: e.activation(out=dec[:], in_=dps[:], func=AF.Exp), reads=[Bdps], writes=[Bdec])
        bsb, Bbsb = bsb_r.next()
        P.op("dve", lambda e: e.tensor_copy(out=bsb[:], in_=bps[:]), reads=[Bbps], writes=[Bbsb])
        eb, Beb = e_r.next()
        P.op("act", lambda e: e.activation(out=eb[:], in_=bsb[:], func=AF.Exp), reads=[Bbsb], writes=[Beb])
        enb, Benb = e_r.next()
        P.op("act", lambda e: e.activation(out=enb[:], in_=bsb[:], func=AF.Exp, scale=-1.0), reads=[Bbsb], writes=[Benb])
        ed, Bed = e_r.next()
        P.op("dve", lambda e: e.tensor_tensor(out=ed[:], in0=bt[:], in1=bsb[:], op=ALU.subtract), reads=[Bbt, Bbsb], writes=[Bed])
        P.op("act", lambda e: e.activation(out=ed[:], in_=ed[:], func=AF.Exp), reads=[Bed], writes=[Bed])
        qt, Bqt = qt_r.next()
        P.op("dve", lambda e: e.tensor_tensor(out=qt[:], in0=xq[:, gi, :], in1=eb[:], op=ALU.mult), reads=[Bxq, Beb], writes=[Bqt])
        kt, Bkt = kt_r.next()
        P.op("pool", lambda e: e.tensor_tensor(out=kt[:], in0=kk[:], in1=enb[:], op=ALU.mult), reads=[Bkk, Benb], writes=[Bkt])
        ke, Bke = ke_r.next()
        P.op("dve", lambda e: e.tensor_tensor(out=ke[:], in0=kk[:], in1=ed[:], op=ALU.mult), reads=[Bkk, Bed], writes=[Bke])
        tp, Btp = tp_r.next()
        qtT, BqtT = qtT_r.next()
        P.op("pe", lambda e: e.transpose(tp[:], qt[:], ident[:]), reads=[Bqt, Bident], writes=[Btp])
        P.op("act", lambda e: e.activation(out=qtT[:], in_=tp[:], func=AF.Identity), reads=[Btp], writes=[BqtT])
        tp2, Btp2 = tp_r.next()
        ktT, BktT = ktT_r.next()
        P.op("pe", lambda e: e.transpose(tp2[:], kt[:], ident[:]), reads=[Bkt, Bident], writes=[Btp2])
        P.op("dve", lambda e: e.tensor_copy(out=ktT[:], in_=tp2[:]), reads=[Btp2], writes=[BktT])
        at, Bat = at_r.next()
        P.op("pe", lambda e: e.matmul(at[:], lhsT=ktT[:], rhs=qtT[:], start=True, stop=True), reads=[BktT, BqtT], writes=[Bat])
        atm, Batm = atm_r.next()
        P.op("dve", lambda e: e.tensor_tensor(out=atm[:], in0=at[:], in1=tri[:], op=ALU.mult), reads=[Bat, Btri], writes=[Batm])
        oT, BoT = oT_r.next()
        vb = xv[:, gi, :]
        P.op("pe", lambda e: e.matmul(oT[:], lhsT=vb, rhs=atm[:], start=True, stop=False), reads=[Bxv, Batm], writes=[BoT])

        def do_chunk(c):
            Sb_, BSb_ = state["Sb"], state["BSb"]
            P.op("pe", lambda e: e.matmul(oT[:, 32 * c:32 * c + 32], lhsT=Sb_[:], rhs=qtT[:, 32 * c:32 * c + 32], start=False, stop=(c == 3)),
                 reads=[BSb_, BqtT], writes=[BoT])
            kem, Bkem = kem_r.next()
            P.op("pool", lambda e: e.tensor_scalar(out=kem[:], in0=ke[:], scalar1=cm[:, c:c + 1], scalar2=None, op0=ALU.mult), reads=[Bke, Bcm], writes=[Bkem])
            u, Bu = u_r.next()
            P.op("pe", lambda e: e.matmul(u[:], lhsT=kem[:], rhs=vb, start=True, stop=True), reads=[Bkem, Bxv], writes=[Bu])
            P.op("dve", lambda e: e.scalar_tensor_tensor(out=S[:], in0=S[:], scalar=dec[:, c:c + 1], in1=u[:], op0=ALU.mult, op1=ALU.add),
                 reads=[BS, Bdec, Bu], writes=[BS])
            Sn, BSn = Sbr.next()
            P.op("act", lambda e: e.activation(out=Sn[:], in_=S[:], func=AF.Identity), reads=[BS], writes=[BSn])
            state["Sb"], state["BSb"] = Sn, BSn

        for c in range(4):
            do_chunk(c)
        if t % 4 == 0:
            state["os"] = hos_r.next()
        os_, Bos = state["os"]
        j = t % 4
        P.op("dve", lambda e: e.tensor_copy(out=os_[:, j * 128:(j + 1) * 128], in_=oT[:]), reads=[BoT], writes=[Bos])
        if j == 3 or t == NKT - 1:
            t0 = t - j
            w = (j + 1) * 128
            P.dma("sp", lambda e: e.dma_start(out=ohT_d[:, t0 * 128:t0 * 128 + w], in_=os_[:, :w]), Bos, reads=[Bos], writes=[Bfin], final=True)

    def h_group(g0, gn):
        xq, Bxq = hqr.next(); xf, Bxf = hfr.next(); xv, Bxv = hvr.next()
        P.dma("sp", lambda e: e.dma_start(out=xq[:, :gn, :], in_=hqv[:, g0:g0 + gn, :]), Bxq, writes=[Bxq])
        P.dma("sp", lambda e: e.dma_start(out=xf[:, :gn, :], in_=hfv[:, g0:g0 + gn, :]), Bxf, writes=[Bxf])
        P.dma("pool", lambda e: e.dma_start(out=xv[:, :gn, :], in_=hvv_[:, g0:g0 + gn, :]), Bxv, writes=[Bxv])
        for gi in range(gn):
            h_tile(xq, Bxq, xf, Bxf, xv, Bxv, gi, g0 + gi)

    if "hgrn" in parts:
        for (g0, gn) in groups(NKT, GR):
            h_group(g0, gn)
    P.emit(); P.close()
    return nc


G_TOK = 8448
N_CTX = 256
N_LAT = 8192


def rope_tables(n_tokens, dim):
    rows = n_tokens // 64
    row = np.repeat(np.arange(rows, dtype=np.float32), 64)
    col = np.tile(np.arange(64, dtype=np.float32), rows)
    axis_dim = dim // 2
    inv_freq = (np.float32(10000.0) ** (-np.arange(0, axis_dim, 2, dtype=np.float32) / np.float32(axis_dim))).astype(np.float32)
    ang = np.concatenate([row[:, None] * inv_freq, col[:, None] * inv_freq], axis=-1).astype(np.float32)
    return np.cos(ang).astype(np.float32), np.sin(ang).astype(np.float32)


def const_tables():
    ca, sa = rope_tables(N_LAT, 128)
    cd, sd = rope_tables(N_LAT, 64)
    cosa = np.concatenate([np.ones((N_CTX, 64), np.float32), ca], 0)
    sina = np.concatenate([np.zeros((N_CTX, 64), np.float32), sa], 0)
    cosd = np.concatenate([np.ones((N_CTX, 64), np.float32), np.concatenate([cd, cd], 1)], 0)
    sind = np.concatenate([np.zeros((N_CTX, 64), np.float32), np.concatenate([sd, sd], 1)], 0)
    s = np.arange(128)
    tri = ((s[:, None] <= s[None, :]) & (s[:, None] // 32 == s[None, :] // 32)).astype(np.float32)
    blk = (s[:, None] // 32 == s[None, :] // 32).astype(np.float32)
    cm = (s[:, None] // 32 == np.arange(4)[None, :]).astype(np.float32)
    ident = np.eye(128, dtype=np.float32)
    return dict(cosa=cosa, sina=sina, cosd=cosd, sind=sind, tri=tri, blk=blk, cm=cm, ident=ident)


def bc(v, n=128):
    return np.ascontiguousarray(np.broadcast_to(np.asarray(v)[None], (n,) + tuple(np.shape(v))))


def dq_rows(half):
    return np.concatenate([np.arange(128 * half, 128 * half + 128), np.arange(N_CTX + 4096 * half, N_CTX + 4096 * half + 4096)])


def scan_rows(direction):
    if direction == 0:
        return np.arange(G_TOK)
    return np.concatenate([np.arange(N_CTX - 1, -1, -1), np.arange(G_TOK - 1, N_CTX - 1, -1)])


def maps_B(Pall, l, inp, C):
    maps = []
    ca = np.ascontiguousarray
    for i in range(8):
        hd, half = i // 2, i % 2
        hh, dr = i % 4, i // 4
        rq = dq_rows(half)
        rs = scan_rows(dr)
        m = dict(
            ident=C["ident"], tri=C["tri"], blk=C["blk"], cm=C["cm"],
            gq=ca(Pall[:, 3072 + 128 * i:3072 + 128 * i + 128]),
            gk=ca(Pall[:, 4096 + 128 * (i // 4):4096 + 128 * (i // 4) + 128]),
            gv=ca(Pall[:, 4352 + 128 * (i // 4):4352 + 128 * (i // 4) + 128]),
            qn=bc(inp["q_norm_g"][l]), kn=bc(inp["k_norm_g"][l]),
            cosa=C["cosa"], sina=C["sina"],
            dq=ca(Pall[rq, 4608 + 128 * hd:4608 + 128 * hd + 128]),
            dk=ca(Pall[:, 5120 + 128 * hd:5120 + 128 * hd + 128]),
            dv=ca(Pall[:, 5632 + 128 * hd:5632 + 128 * hd + 128]),
            cosdq=ca(C["cosd"][rq]), sindq=ca(C["sind"][rq]), cosdk=C["cosd"], sindk=C["sind"],
            lamv=bc(np.stack([inp["lam_q1"][l], inp["lam_k1"][l], inp["lam_q2"][l], inp["lam_k2"][l]], 0)),
            dn=ca(inp["diff_norm_g"][l][:, None]),
            hq=ca(Pall[rs, 512 * dr + 128 * hh:512 * dr + 128 * hh + 128]),
            hf=ca(Pall[rs, 1024 + 512 * dr + 128 * hh:1024 + 512 * dr + 128 * hh + 128]),
            hv=ca(Pall[rs, 2048 + 128 * hh:2048 + 128 * hh + 128]),
            lbl=bc(inp["hg_lb_logits"][:, 128 * hh:128 * hh + 128]),
        )
        maps.append(m)
    return maps


def build_C(T, NL):
    nc = new_nc()
    ohf_d = din(nc, "ohf", [4, 128, T]); ohb_d = din(nc, "ohb", [4, 128, T]); gate_d = din(nc, "gate", [4, 128, T])
    att_d = din(nc, "att", [8, 128, T]); dif_d = din(nc, "dif", [4, 128, T])
    hgn_d = din(nc, "hgn", [128, 1])
    wo_d = din(nc, "wo", [2048, 2048])
    xT_d = din(nc, "xT", [128, NCH, T])
    mod_d = din(nc, "mod", [128, 96, 2])
    g_d = din(nc, "g2", [128, NCH])
    rw_d = din(nc, "rw", [2048, 36]); rb_d = din(nc, "rb", [128, 36])
    x1_d = dout(nc, "x1T", [128, NCH, T])
    h2_d = dout(nc, "h2T", [128, NCH, T], BF16)
    cb_d = dout(nc, "comb", [T, 32])
    P = Prog(nc)
    Bfin = P.buf("fin")
    ones, Bones = load_consts(P, T)
    xT = P.sbuf("xT", [128, NCH, T], F32); Bx = P.buf("xT")
    mod = P.sbuf("mod", [128, 96, 2], F32); Bmod = P.buf("mod")
    g = P.sbuf("g", [128, NCH], F32); Bg = P.buf("g")
    hgn = P.sbuf("hgn", [128, 1], F32); Bhgn = P.buf()
    rw = P.sbuf("rw", [128, NCH, 36], F32); Brw = P.buf()
    rb = P.sbuf("rb", [128, 36], F32); Brb = P.buf()
    mixT = P.sbuf("mixT", [128, NCH, T], BF16); Bmix = P.buf()
    P.dma("sp", [lambda e, q=q: e.dma_start(out=xT[:, q * 4:(q + 1) * 4, :], in_=xT_d[:, q * 4:(q + 1) * 4, :]) for q in range(4)], Bx, writes=[Bx])
    P.dma("sp", lambda e: e.dma_start(out=mod[:], in_=mod_d[:, :, :]), Bmod, writes=[Bmod])
    P.dma("sp", lambda e: e.dma_start(out=g[:], in_=g_d[:, :]), Bg, writes=[Bg])
    P.dma("sp", lambda e: e.dma_start(out=hgn[:], in_=hgn_d[:, :]), Bhgn, writes=[Bhgn])
    P.dma("sp", lambda e: e.dma_start(out=rw[:], in_=rw_d.rearrange("(c p) n -> p c n", p=128)), Brw, writes=[Brw])
    P.dma("sp", lambda e: e.dma_start(out=rb[:], in_=rb_d[:, :]), Brb, writes=[Brb])
    wov = wo_d.rearrange("(c p) n -> p c n", p=128)
    wor = Ring(P, "wo", 2, [128, NCH, 128], BF16)
    inr = Ring(P, "c_in", 3, [128, 512], F32)
    tr = Ring(P, "c_t", 3, [128, 512], F32)
    sqr = Ring(P, "c_sq", 2, [128, 512], BF16)
    pss = Ring(P, "c_ps", 2, [128, 512], F32, psum=True)

    def hg_block(h, c0, n):
        a_, Ba_ = inr.next(); b_, Bb_ = inr.next(); gt, Bgt = inr.next()
        P.dma("sp", lambda e: e.dma_start(out=a_[:, :n], in_=ohf_d[h, :, c0:c0 + n]), Ba_, writes=[Ba_])
        P.dma("sp", lambda e: e.dma_start(out=b_[:, :n], in_=ohb_d[h, :, c0:c0 + n]), Bb_, writes=[Bb_])
        P.dma("sp", lambda e: e.dma_start(out=gt[:, :n], in_=gate_d[h, :, c0:c0 + n]), Bgt, writes=[Bgt])
        o, Bo = tr.next()
        P.op("dve", lambda e: e.tensor_tensor(out=o[:, :n], in0=a_[:, :n], in1=b_[:, :n], op=ALU.add), reads=[Ba_, Bb_], writes=[Bo])
        sq, Bsq = sqr.next()
        P.op("act", lambda e: e.activation(out=sq[:, :n], in_=o[:, :n], func=AF.Square), reads=[Bo], writes=[Bsq])
        ss, Bss = pss.next()
        P.op("pe", lambda e: e.matmul(ss[:, :n], lhsT=ones[:], rhs=sq[:, :n], start=True, stop=True), reads=[Bsq, Bones], writes=[Bss])
        rs, Brs = tr.next()
        P.op("dve", lambda e: e.tensor_scalar(out=rs[:, :n], in0=ss[:, :n], scalar1=1.0 / 128, scalar2=EPS, op0=ALU.mult, op1=ALU.add), reads=[Bss], writes=[Brs])
        P.op("act", lambda e: e.activation(out=rs[:, :n], in_=rs[:, :n], func=AF.Sqrt), reads=[Brs], writes=[Brs])
        P.op("dve", lambda e: e.reciprocal(out=rs[:, :n], in_=rs[:, :n]), reads=[Brs], writes=[Brs])
        P.op("dve", lambda e: e.scalar_tensor_tensor(out=o[:, :n], in0=o[:, :n], scalar=hgn[:, 0:1], in1=rs[:, :n], op0=ALU.mult, op1=ALU.mult),
             reads=[Bo, Bhgn, Brs], writes=[Bo])
        sg, Bsg = tr.next()
        P.op("act", lambda e: e.activation(out=sg[:, :n], in_=gt[:, :n], func=AF.Sigmoid), reads=[Bgt], writes=[Bsg])
        P.op("pool", lambda e: e.tensor_tensor(out=sg[:, :n], in0=sg[:, :n], in1=gt[:, :n], op=ALU.mult), reads=[Bsg, Bgt], writes=[Bsg])
        P.op("dve", lambda e: e.tensor_tensor(out=mixT[:, h, c0:c0 + n], in0=o[:, :n], in1=sg[:, :n], op=ALU.mult), reads=[Bo, Bsg], writes=[Bmix])

    def cp_block(src_d, hsrc, cdst, c0, n, k):
        a_, Ba_ = inr.next()
        P.dma("sp", lambda e: e.dma_start(out=a_[:, :n], in_=src_d[hsrc, :, c0:c0 + n]), Ba_, writes=[Ba_])
        if k % 2 == 0:
            P.op("act", lambda e: e.activation(out=mixT[:, cdst, c0:c0 + n], in_=a_[:, :n], func=AF.Identity), reads=[Ba_], writes=[Bmix])
        else:
            P.op("pool", lambda e: e.tensor_copy(out=mixT[:, cdst, c0:c0 + n], in_=a_[:, :n]), reads=[Ba_], writes=[Bmix])

    k = 0
    for (c0, n, j) in blocks(T, NL):
        for h in range(4):
            hg_block(h, c0, n)
        for h in range(8):
            cp_block(att_d, h, 4 + h, c0, n, k); k += 1
        for h in range(4):
            cp_block(dif_d, h, 12 + h, c0, n, k); k += 1
    x1T = P.sbuf("x1T", [128, NCH, T], F32); Bx1 = P.buf("x1T")

    def op_block(c0, n, j, oc, wo, Bwo):
        ps, Bps = pss.next()
        for c in range(NCH):
            P.op("pe", lambda e, c=c: e.matmul(ps[:, :n], lhsT=wo[:, c, :], rhs=mixT[:, c, c0:c0 + n], start=(c == 0), stop=(c == NCH - 1)),
                 reads=[Bwo, Bmix], writes=[Bps])
        P.op("dve", lambda e: e.scalar_tensor_tensor(out=x1T[:, oc, c0:c0 + n], in0=ps[:, :n], scalar=mod[:, 32 + oc, j:j + 1], in1=xT[:, oc, c0:c0 + n],
                                                     op0=ALU.mult, op1=ALU.add), reads=[Bps, Bmod, Bx], writes=[Bx1])

    def op_oc(oc):
        wo, Bwo = wor.next()
        P.dma("pool", lambda e: e.dma_start(out=wo[:], in_=wov[:, :, oc * 128:(oc + 1) * 128]), Bwo, writes=[Bwo])
        for (c0, n, j) in blocks(T, NL):
            op_block(c0, n, j, oc, wo, Bwo)

    for oc in range(NCH):
        op_oc(oc)
    P.dma("sp", [lambda e, q=q: e.dma_start(out=x1_d[:, q * 4:(q + 1) * 4, :], in_=x1T[:, q * 4:(q + 1) * 4, :]) for q in range(4)], Bx1, reads=[Bx1], writes=[Bfin], final=True)
    a2, Ba2, b2, Bb2 = mod_ab(P, mod, Bmod, g, Bg, 3, 4, "m2")
    h2T = mixT; Bh2 = Bmix
    h2F = xT; Bh2F = Bx
    norm_mod(P, x1T, Bx1, a2, Ba2, b2, Bb2, h2T, Bh2, T, NL, ones, Bones, hF=h2F, BhF=Bh2F)
    P.dma("sp", [lambda e, q=q: e.dma_start(out=h2_d[:, q * 4:(q + 1) * 4, :], in_=h2T[:, q * 4:(q + 1) * 4, :]) for q in range(4)], Bh2, reads=[Bh2], writes=[Bfin], final=True)
    sm = Ring(P, "r_sm", 24, [128, 8], F32)
    lgr = Ring(P, "r_lg", 2, [128, 36], F32)
    cbr = Ring(P, "r_cb", 2, [128, 32], F32)

    def router_tile(m0, mt):
        ps, Bps = pss.next()
        for c in range(NCH):
            P.op("pe", lambda e, c=c: e.matmul(ps[:mt, 0:36], lhsT=h2F[:, c, m0:m0 + mt], rhs=rw[:, c, :], start=(c == 0), stop=(c == NCH - 1)),
                 reads=[Bh2F, Brw], writes=[Bps])
        lg, Blg = lgr.next()
        P.op("dve", lambda e: e.tensor_tensor(out=lg[:mt, :], in0=ps[:mt, 0:36], in1=rb[:mt, :], op=ALU.add), reads=[Bps, Brb], writes=[Blg])

        def T8():
            return sm.next()
        gmax, Bgmax = T8(); ngmax, Bng = T8(); eg, Beg = T8(); gsum, Bgs = T8(); gtop, Bgt = T8(); oh, Boh = T8()
        P.op("dve", lambda e: e.reduce_max(out=gmax[:mt, 0:1], in_=lg[:mt, 0:4], axis=AX.X), reads=[Blg], writes=[Bgmax])
        P.op("dve", lambda e: e.tensor_scalar(out=ngmax[:mt, 0:1], in0=gmax[:mt, 0:1], scalar1=-1.0, scalar2=None, op0=ALU.mult), reads=[Bgmax], writes=[Bng])
        P.op("act", lambda e: e.activation(out=eg[:mt, 0:4], in_=lg[:mt, 0:4], func=AF.Exp, bias=ngmax[:mt, 0:1]), reads=[Blg, Bng], writes=[Beg])
        P.op("dve", lambda e: e.reduce_sum(out=gsum[:mt, 0:1], in_=eg[:mt, 0:4], axis=AX.X), reads=[Beg], writes=[Bgs])
        P.op("dve", lambda e: e.reciprocal(out=gtop[:mt, 0:1], in_=gsum[:mt, 0:1]), reads=[Bgs], writes=[Bgt])
        P.op("dve", lambda e: e.tensor_scalar(out=oh[:mt, 0:4], in0=lg[:mt, 0:4], scalar1=gmax[:mt, 0:1], scalar2=None, op0=ALU.is_equal), reads=[Blg, Bgmax], writes=[Boh])
        es, Bes = T8()
        P.op("dve", lambda e: e.tensor_scalar(out=es[:mt, :], in0=lg[:mt, 4:12], scalar1=oh[:mt, 0:1], scalar2=None, op0=ALU.mult), reads=[Blg, Boh], writes=[Bes])
        for gi in range(1, 4):
            P.op("dve", lambda e, gi=gi: e.scalar_tensor_tensor(out=es[:mt, :], in0=lg[:mt, 4 + 8 * gi:12 + 8 * gi], scalar=oh[:mt, gi:gi + 1], in1=es[:mt, :],
                                                              op0=ALU.mult, op1=ALU.add), reads=[Blg, Boh, Bes], writes=[Bes])
        m1, Bm1 = T8(); eq1, Beq1 = T8(); e2, Be2 = T8(); m2, Bm2 = T8(); eq2, Beq2 = T8()
        P.op("dve", lambda e: e.reduce_max(out=m1[:mt, 0:1], in_=es[:mt, :], axis=AX.X), reads=[Bes], writes=[Bm1])
        P.op("dve", lambda e: e.tensor_scalar(out=eq1[:mt, :], in0=es[:mt, :], scalar1=m1[:mt, 0:1], scalar2=None, op0=ALU.is_equal), reads=[Bes, Bm1], writes=[Beq1])
        P.op("dve", lambda e: e.scalar_tensor_tensor(out=e2[:mt, :], in0=eq1[:mt, :], scalar=-1e30, in1=es[:mt, :], op0=ALU.mult, op1=ALU.add),
             reads=[Beq1, Bes], writes=[Be2])
        P.op("dve", lambda e: e.reduce_max(out=m2[:mt, 0:1], in_=e2[:mt, :], axis=AX.X), reads=[Be2], writes=[Bm2])
        P.op("dve", lambda e: e.tensor_scalar(out=eq2[:mt, :], in0=e2[:mt, :], scalar1=m2[:mt, 0:1], scalar2=None, op0=ALU.is_equal), reads=[Be2, Bm2], writes=[Beq2])
        r, Br = T8(); w1, Bw1 = T8(); w2, Bw2 = T8(); c8, Bc8 = T8()
        P.op("dve", lambda e: e.tensor_tensor(out=r[:mt, 0:1], in0=m2[:mt, 0:1], in1=m1[:mt, 0:1], op=ALU.subtract), reads=[Bm1, Bm2], writes=[Br])
        P.op("act", lambda e: e.activation(out=r[:mt, 0:1], in_=r[:mt, 0:1], func=AF.Exp), reads=[Br], writes=[Br])
        P.op("dve", lambda e: e.tensor_scalar(out=w1[:mt, 0:1], in0=r[:mt, 0:1], scalar1=1.0, scalar2=None, op0=ALU.add), reads=[Br], writes=[Bw1])
        P.op("dve", lambda e: e.reciprocal(out=w1[:mt, 0:1], in_=w1[:mt, 0:1]), reads=[Bw1], writes=[Bw1])
        P.op("dve", lambda e: e.tensor_tensor(out=w1[:mt, 0:1], in0=w1[:mt, 0:1], in1=gtop[:mt, 0:1], op=ALU.mult), reads=[Bw1, Bgt], writes=[Bw1])
        P.op("dve", lambda e: e.tensor_tensor(out=w2[:mt, 0:1], in0=w1[:mt, 0:1], in1=r[:mt, 0:1], op=ALU.mult), reads=[Bw1, Br], writes=[Bw2])
        P.op("dve", lambda e: e.tensor_scalar(out=c8[:mt, :], in0=eq1[:mt, :], scalar1=w1[:mt, 0:1], scalar2=None, op0=ALU.mult), reads=[Beq1, Bw1], writes=[Bc8])
        P.op("dve", lambda e: e.scalar_tensor_tensor(out=c8[:mt, :], in0=eq2[:mt, :], scalar=w2[:mt, 0:1], in1=c8[:mt, :], op0=ALU.mult, op1=ALU.add),
             reads=[Beq2, Bw2, Bc8], writes=[Bc8])
        cb, Bcb = cbr.next()
        for gi in range(4):
            P.op("dve", lambda e, gi=gi: e.tensor_scalar(out=cb[:mt, 8 * gi:8 * gi + 8], in0=c8[:mt, :], scalar1=oh[:mt, gi:gi + 1], scalar2=None, op0=ALU.mult),
                 reads=[Bc8, Boh, Bcb] if gi else [Bc8, Boh], writes=[Bcb])
        P.dma("sp", lambda e: e.dma_start(out=cb_d[m0:m0 + mt, :], in_=cb[:mt, :]), Bcb, reads=[Bcb], writes=[Bfin], final=True)

    for m0 in range(0, T, 128):
        router_tile(m0, min(128, T - m0))
    P.emit(); P.close()
    return nc


def tok_idx(j):
    return np.concatenate([np.arange(N_CTX + 1024 * j, N_CTX + 1024 * (j + 1)), np.arange(32 * j, 32 * (j + 1))])


def fm(a):
    T = a.shape[0]
    return np.ascontiguousarray(a.T.reshape(16, 128, T).transpose(1, 0, 2))


def unfm(a):
    T = a.shape[2]
    return np.ascontiguousarray(a.transpose(1, 0, 2).reshape(2048, T).T)


def assemble_mix_inputs(Bres, Pall):
    att = np.stack([Bres[i]["attT"] for i in range(8)], 0)
    dif = np.empty((4, 128, G_TOK), np.float32)
    for i in range(8):
        dif[i // 2][:, dq_rows(i % 2)] = Bres[i]["diffT"]
    ohf = np.stack([Bres[h]["ohT"] for h in range(4)], 0)
    ohb = np.empty((4, 128, G_TOK), np.float32)
    rs = scan_rows(1)
    for h in range(4):
        ohb[h][:, rs] = Bres[4 + h]["ohT"]
    gate = np.ascontiguousarray(Pall[:, 2560:3072].T.reshape(4, 128, G_TOK))
    return att, dif, ohf, ohb, gate


def maps_C(att, dif, ohf, ohb, gate, xTs, modT, l, inp):
    ca = np.ascontiguousarray
    rw = ca(np.concatenate([inp["router_group_w"][l], inp["router_expert_w"][l]], 1))
    rb = bc(np.concatenate([inp["router_group_b"][l], inp["router_expert_b"][l]], 0))
    maps = []
    for j in range(8):
        ti = tok_idx(j)
        maps.append(dict(ohf=ca(ohf[:, :, ti]), ohb=ca(ohb[:, :, ti]), gate=ca(gate[:, :, ti]), att=ca(att[:, :, ti]), dif=ca(dif[:, :, ti]),
                         hgn=ca(inp["hg_norm_g"][l][:, None]), wo=inp["w_out"][l], xT=xTs[j], mod=modT,
                         g2=ca(inp["norm2_g"][l].reshape(16, 128).T), rw=rw, rb=rb))
    return maps


def build_D(G=G_TOK):
    nc = new_nc()
    h2_d = din(nc, "h2T", [128, NCH, G], BF16)
    cb_d = din(nc, "cbb", [4, 128, G])
    wg_d = din(nc, "wg", [4, 2048, 512]); wu_d = din(nc, "wu", [4, 2048, 512]); wd_d = din(nc, "wd", [4, 512, 2048])
    yp_d = dout(nc, "ypT", [128, NCH, G])
    wgb = nc.dram_tensor("wgb", [4, 128, NCH, 512], BF16).ap()
    wub = nc.dram_tensor("wub", [4, 128, NCH, 512], BF16).ap()
    wdb = nc.dram_tensor("wdb", [4, 128, 4, 2048], BF16).ap()
    P = Prog(nc)
    Bfin = P.buf("fin")
    wgr = Ring(P, "wg", 2, [128, NCH, 512], BF16)
    wur = Ring(P, "wu", 2, [128, NCH, 512], BF16)
    wdr = Ring(P, "wd", 2, [128, 4, 2048], BF16)
    Bs = {k: [P.buf(f"{k}{e}") for e in range(4)] for k in ("g", "u", "d")}

    def prepass(e):
        for (key, src, dst, ring, pat) in (("g", wg_d, wgb, wgr, "(c p) n -> p c n"), ("u", wu_d, wub, wur, "(c p) n -> p c n"), ("d", wd_d, wdb, wdr, "(c p) n -> p c n")):
            t, Bt = ring.next()
            srcv = src[e].rearrange(pat, p=128)
            P.dma("pool", lambda e_, t=t, srcv=srcv: e_.dma_start(out=t[:], in_=srcv), Bt, writes=[Bt])
            P.dma("sp", lambda e_, t=t, dst=dst: e_.dma_start(out=dst[e], in_=t[:]), Bt, reads=[Bt], writes=[Bs[key][e]])

    for e in range(4):
        prepass(e)
    h2r = Ring(P, "h2", 2, [128, NCH, 512], BF16)
    cbr = Ring(P, "cb", 1, [128, 4, 512], F32)
    aTr = Ring(P, "aT", 2, [128, 4, 512], BF16)
    sgr = Ring(P, "sg", 2, [128, 512], F32)
    tr = Ring(P, "tt", 2, [128, 512], F32)
    yacc = P.sbuf("yacc", [128, NCH, 512], F32); Byacc = P.buf()
    gps = Ring(P, "gps", 2, [128, 512], F32, psum=True)
    ups = Ring(P, "ups", 2, [128, 512], F32, psum=True)
    dps = Ring(P, "dps", 2, [128, 512], F32, psum=True)
    cbv = cb_d.rearrange("e p g -> p e g")

    def do_expert(e, h2, Bh2, cb, Bcb, c0, n):
        wg, Bwg = wgr.next(); wu, Bwu = wur.next(); wd, Bwd = wdr.next()
        P.dma("sp", lambda e_: e_.dma_start(out=wg[:], in_=wgb[e]), Bwg, reads=[Bs["g"][e]], writes=[Bwg])
        P.dma("act", lambda e_: e_.dma_start(out=wu[:], in_=wub[e]), Bwu, reads=[Bs["u"][e]], writes=[Bwu])
        P.dma("sp", lambda e_: e_.dma_start(out=wd[:], in_=wdb[e]), Bwd, reads=[Bs["d"][e]], writes=[Bwd])
        aT, BaT = aTr.next()

        def do_fc(fc):
            gp, Bgp = gps.next(); up, Bup = ups.next()
            for c in range(NCH):
                P.op("pe", lambda e_, c=c: e_.matmul(gp[:, :n], lhsT=wg[:, c, fc * 128:(fc + 1) * 128], rhs=h2[:, c, :n], start=(c == 0), stop=(c == NCH - 1)),
                     reads=[Bwg, Bh2], writes=[Bgp])
            for c in range(NCH):
                P.op("pe", lambda e_, c=c: e_.matmul(up[:, :n], lhsT=wu[:, c, fc * 128:(fc + 1) * 128], rhs=h2[:, c, :n], start=(c == 0), stop=(c == NCH - 1)),
                     reads=[Bwu, Bh2], writes=[Bup])
            sg, Bsg = sgr.next()
            P.op("act", lambda e_: e_.activation(out=sg[:, :n], in_=gp[:, :n], func=AF.Sigmoid), reads=[Bgp], writes=[Bsg])
            P.op("dve", lambda e_: e_.tensor_tensor(out=sg[:, :n], in0=sg[:, :n], in1=gp[:, :n], op=ALU.mult), reads=[Bsg, Bgp], writes=[Bsg])
            tt, Btt = tr.next()
            P.op("dve", lambda e_: e_.tensor_tensor(out=tt[:, :n], in0=sg[:, :n], in1=up[:, :n], op=ALU.mult), reads=[Bsg, Bup], writes=[Btt])
            P.op("pool", lambda e_: e_.tensor_tensor(out=aT[:, fc, :n], in0=tt[:, :n], in1=cb[:, e, :n], op=ALU.mult), reads=[Btt, Bcb], writes=[BaT])

        for fc in range(4):
            do_fc(fc)

        def do_oc(oc):
            dp, Bdp = dps.next()
            for fc in range(4):
                P.op("pe", lambda e_, fc=fc: e_.matmul(dp[:, :n], lhsT=wd[:, fc, oc * 128:(oc + 1) * 128], rhs=aT[:, fc, :n], start=(fc == 0), stop=(fc == 3)),
                     reads=[Bwd, BaT], writes=[Bdp])
            if e == 0:
                P.op("act", lambda e_: e_.activation(out=yacc[:, oc, :n], in_=dp[:, :n], func=AF.Identity), reads=[Bdp], writes=[Byacc])
            else:
                P.op("dve", lambda e_: e_.tensor_tensor(out=yacc[:, oc, :n], in0=yacc[:, oc, :n], in1=dp[:, :n], op=ALU.add), reads=[Bdp, Byacc], writes=[Byacc])

        for oc in range(NCH):
            do_oc(oc)

    def do_block(c0, n):
        h2, Bh2 = h2r.next(); cb, Bcb = cbr.next()
        P.dma("sp", lambda e_: e_.dma_start(out=h2[:, :, :n], in_=h2_d[:, :, c0:c0 + n]), Bh2, writes=[Bh2])
        P.dma("act", lambda e_: e_.dma_start(out=cb[:, :, :n], in_=cbv[:, :, c0:c0 + n]), Bcb, writes=[Bcb])
        for e in range(4):
            do_expert(e, h2, Bh2, cb, Bcb, c0, n)
        P.dma("sp", [lambda e_, q=q: e_.dma_start(out=yp_d[:, q * 4:(q + 1) * 4, c0:c0 + n], in_=yacc[:, q * 4:(q + 1) * 4, :n]) for q in range(4)],
              Byacc, reads=[Byacc], writes=[Bfin], final=True)

    for (c0, n) in groups(G, 512):
        do_block(c0, n)
    P.emit(); P.close()
    return nc


def build_E(T, NL, final):
    nc = new_nc()
    x1_d = din(nc, "x1T", [128, NCH, T])
    yp_d = din(nc, "yp", [8, 128, NCH, T])
    mod_d = din(nc, "mod", [128, 96, 2])
    x2_d = dout(nc, "x2T", [128, NCH, T])
    P = Prog(nc)
    Bfin = P.buf("fin")
    x1T = P.sbuf("x1T", [128, NCH, T], F32); Bx1 = P.buf()
    mod = P.sbuf("mod", [128, 96, 2], F32); Bmod = P.buf()
    P.dma("sp", [lambda e, q=q: e.dma_start(out=x1T[:, q * 4:(q + 1) * 4, :], in_=x1_d[:, q * 4:(q + 1) * 4, :]) for q in range(4)], Bx1, writes=[Bx1])
    P.dma("sp", lambda e: e.dma_start(out=mod[:], in_=mod_d[:, :, :]), Bmod, writes=[Bmod])
    ldr = Ring(P, "ld", 4, [128, T], F32)
    accr = Ring(P, "acc", 2, [128, T], F32)
    x2T = P.sbuf("x2T", [128, NCH, T], F32); Bx2 = P.buf()

    def do_oc(oc):
        acc, Bacc = accr.next()
        for i in range(8):
            t, Bt = ldr.next()
            P.dma("sp" if i % 2 == 0 else "act", lambda e, t=t, i=i: e.dma_start(out=t[:], in_=yp_d[i, :, oc, :]), Bt, writes=[Bt])
            if i == 0:
                P.op("pool", lambda e, t=t: e.tensor_copy(out=acc[:], in_=t[:]), reads=[Bt], writes=[Bacc])
            else:
                P.op("dve" if i % 2 else "pool", lambda e, t=t: e.tensor_tensor(out=acc[:], in0=acc[:], in1=t[:], op=ALU.add), reads=[Bt, Bacc], writes=[Bacc])
        for (c0, n, j) in ((0, NL, 0), (NL, T - NL, 1)):
            P.op("dve", lambda e, c0=c0, n=n, j=j: e.scalar_tensor_tensor(out=x2T[:, oc, c0:c0 + n], in0=acc[:, c0:c0 + n], scalar=mod[:, 80 + oc, j:j + 1],
                                                                      in1=x1T[:, oc, c0:c0 + n], op0=ALU.mult, op1=ALU.add),
                 reads=[Bacc, Bmod, Bx1], writes=[Bx2])

    for oc in range(NCH):
        do_oc(oc)
    P.dma("sp", [lambda e, q=q: e.dma_start(out=x2_d[:, q * 4:(q + 1) * 4, :], in_=x2T[:, q * 4:(q + 1) * 4, :]) for q in range(4)], Bx2, reads=[Bx2], writes=[Bfin], final=True)
    if final:
        fg_d = din(nc, "fg", [128, NCH])
        out_d = dout(nc, "outT", [128, NCH, T])
        fg = P.sbuf("fg", [128, NCH], F32); Bfg = P.buf()
        P.dma("sp", lambda e: e.dma_start(out=fg[:], in_=fg_d[:, :]), Bfg, writes=[Bfg])
        ones, Bones = load_consts(P, T)
        a = P.sbuf("fa", [128, NCH, 2], F32); Ba = P.buf()
        b = P.sbuf("fb", [128, NCH, 2], F32); Bb = P.buf()
        for j in range(2):
            P.op("dve", lambda e, j=j: e.tensor_copy(out=a[:, :, j], in_=fg[:, :]), reads=[Bfg], writes=[Ba])
        P.op("dve", lambda e: e.memset(b[:], 0.0), writes=[Bb])
        hT = P.sbuf("fhT", [128, NCH, T], BF16); Bh = P.buf()
        outF = x1T; BoutF = Bx1
        norm_mod(P, x2T, Bx2, a, Ba, b, Bb, hT, Bh, T, NL, ones, Bones, hF=outF, BhF=BoutF)
        P.dma("sp", [lambda e, q=q: e.dma_start(out=out_d[:, q * 4:(q + 1) * 4, :], in_=outF[:, q * 4:(q + 1) * 4, :]) for q in range(4)], BoutF, reads=[BoutF], writes=[Bfin], final=True)
    P.emit(); P.close()
    return nc


def assemble_global(parts, last_axis=True):
    if last_axis:
        out = np.empty(parts[0].shape[:-1] + (G_TOK,), parts[0].dtype)
        for j in range(8):
            out[..., tok_idx(j)] = parts[j]
    else:
        out = np.empty((G_TOK,) + parts[0].shape[1:], parts[0].dtype)
        for j in range(8):
            out[tok_idx(j)] = parts[j]
    return out


def maps_D(h2_full, comb_full, l, inp):
    ca = np.ascontiguousarray
    maps = []
    for i in range(8):
        cbb = ca(np.broadcast_to(comb_full[:, 4 * i:4 * i + 4].T[:, None, :], (4, 128, G_TOK)))
        maps.append(dict(h2T=h2_full, cbb=cbb, wg=inp["w_gate"][l, 4 * i:4 * i + 4], wu=inp["w_up"][l, 4 * i:4 * i + 4], wd=inp["w_down"][l, 4 * i:4 * i + 4]))
    return maps


_PROGS = {}


def _prog(key, builder):
    if key not in _PROGS:
        _PROGS[key] = builder()
    return _PROGS[key]


def _run(nc, maps):
    return run_bass_kernel_spmd(nc, maps, core_ids=list(range(8))).results


def kernel(**inputs):
    import math
    inp = {k: np.asarray(v) for k, v in inputs.items()}
    ca = np.ascontiguousarray
    C = const_tables()
    T, NL = 1056, 1024
    cc = ca(np.stack([inp["c"][0], inp["c_ctx"]], -1).reshape(16, 128, 2).transpose(1, 0, 2))
    mapsM = []
    for i in range(8):
        mapsM.append(dict(cc=cc, wm=ca(inp["w_mod"][:, :, i * 1536:(i + 1) * 1536]),
                          bm=ca(inp["b_mod"][:, i * 1536:(i + 1) * 1536].reshape(2, 12, 128).transpose(0, 2, 1))))
    resM = _run(_prog("M", build_M), mapsM)
    mo = np.stack([r["mo"] for r in resM], 0)
    mod = mo.transpose(1, 0, 3, 2, 4).reshape(2, 12288, 2)
    del mapsM
    xg = np.concatenate([inp["ctx"][0], inp["x"][0]], 0)
    xTs = [fm(xg[tok_idx(j)]) for j in range(8)]
    out = None
    for l in range(2):
        final = (l == 1)
        modT = ca(mod[l].reshape(96, 128, 2).transpose(1, 0, 2))
        g1 = ca(inp["norm1_g"][l].reshape(16, 128).T)
        resA = _run(_prog("A", lambda: build_A(T, NL)), [dict(xT=xTs[j], mod=modT, g1=g1, w=inp["w_in"][l]) for j in range(8)])
        Pall = assemble_global([r["p"] for r in resA], last_axis=False)
        del resA
        lam_init = 0.8 - 0.6 * math.exp(-0.3 * l)
        resB = _run(_prog(("B", l), lambda: build_B(l, lam_init=lam_init)), maps_B(Pall, l, inp, C))
        att, dif, ohf, ohb, gate = assemble_mix_inputs(resB, Pall)
        del resB, Pall
        resC = _run(_prog("C", lambda: build_C(T, NL)), maps_C(att, dif, ohf, ohb, gate, xTs, modT, l, inp))
        del att, dif, ohf, ohb, gate
        x1Ts = [r["x1T"] for r in resC]
        h2_full = assemble_global([r["h2T"] for r in resC])
        comb_full = assemble_global([r["comb"] for r in resC], last_axis=False)
        del resC
        resD = _run(_prog("D", build_D), maps_D(h2_full, comb_full, l, inp))
        yps = [r["ypT"] for r in resD]
        del resD, h2_full
        mapsE = []
        for j in range(8):
            ti = tok_idx(j)
            m = dict(x1T=x1Ts[j], yp=ca(np.stack([yps[i][:, :, ti] for i in range(8)], 0)), mod=modT)
            if final:
                m["fg"] = ca(inp["final_norm_g"].reshape(16, 128).T)
            mapsE.append(m)
        del yps
        resE = _run(_prog(("E", final), lambda: build_E(T, NL, final)), mapsE)
        del mapsE
        xTs = [r["x2T"] for r in resE]
        if final:
            out = np.concatenate([unfm(r["outT"])[:NL] for r in resE], 0)[None]
    return np.ascontiguousarray(out.astype(np.float32))
```
